# Optimizing a Trainium2 kernel written in Bass

```python
import math
import jax
import jax.numpy as jnp
from jax import lax
import numpy as np

D_MODEL = 2048
BATCH = 1
SEQ = 16384
DEPTH = 2

N_META = 16
ATTN_HEADS = 8
ATTN_KV_HEADS = 2
ATTN_GROUP = ATTN_HEADS // ATTN_KV_HEADS
HEAD_DIM = 128
WINDOW = 128
BLOCK = 128
ROPE_THETA = 10000.0
Q_DIM = ATTN_HEADS * HEAD_DIM
KV_DIM = ATTN_KV_HEADS * HEAD_DIM
S5_WIDTH = D_MODEL // 2
S5_GROUP = 16
S5_GROUPS = S5_WIDTH // S5_GROUP
S5_STATE = 64
AB_IN_DIM = Q_DIM + 2 * KV_DIM + S5_WIDTH
AB_OUT_DIM = Q_DIM + S5_WIDTH
HG_HEADS = 16
HG_DK = D_MODEL // HG_HEADS
HG_DV = D_MODEL // HG_HEADS
HG_CHUNK = 64
C_IN_DIM = 5 * D_MODEL
N_GROUPS = 8
EXPERTS_PER_GROUP = 8
N_EXPERTS = N_GROUPS * EXPERTS_PER_GROUP
TOP_K = 2
D_EXPERT = 512
MOE_BLOCK = 128
N_AB_LAYERS = (DEPTH + 1) // 2
N_C_LAYERS = DEPTH // 2
DN_ALPHA = (2.0 * DEPTH) ** 0.25
DN_BETA = (8.0 * DEPTH) ** -0.25
LN_EPS = 1e-5
RMS_EPS = 1e-6
NEG_INF = -1e30

kernel_name = 'hybrid_swa_s5_hgrn2_hmoe_encoder'


def layer_norm(x, g, b):
    xf = x.astype(jnp.float32)
    mu = jnp.mean(xf, axis=-1, keepdims=True)
    var = jnp.mean(jnp.square(xf - mu), axis=-1, keepdims=True)
    return ((xf - mu) * lax.rsqrt(var + LN_EPS) * g + b).astype(x.dtype)


def rope(x, pos):
    half = HEAD_DIM // 2
    inv = ROPE_THETA ** (-jnp.arange(half, dtype=jnp.float32) * 2.0 / HEAD_DIM)
    ang = pos.astype(jnp.float32)[:, None] * inv[None, :]
    cos = jnp.cos(ang)[None, :, None, :]
    sin = jnp.sin(ang)[None, :, None, :]
    xf = x.astype(jnp.float32)
    x1, x2 = xf[..., :half], xf[..., half:]
    return jnp.concatenate([x1 * cos - x2 * sin, x2 * cos + x1 * sin], axis=-1).astype(x.dtype)


def to_padded(t, front):
    pad = jnp.zeros((t.shape[0], front - N_META) + t.shape[2:], t.dtype)
    return jnp.concatenate([t[:, :N_META], pad, t[:, N_META:]], axis=1)


def from_padded(t, front):
    return jnp.concatenate([t[:, :N_META], t[:, front:]], axis=1)


def window_attention(q, k, v, sinks):
    B = q.shape[0]
    qp, kp, vp = to_padded(q, BLOCK), to_padded(k, BLOCK), to_padded(v, BLOCK)
    lp = qp.shape[1]
    nb = lp // BLOCK
    slot = jnp.arange(lp)
    lpos = jnp.where(slot < N_META, slot, slot - (BLOCK - N_META))
    is_real = slot >= BLOCK

    def band(t, axis):
        widths = [(0, 0)] * t.ndim
        widths[axis] = (BLOCK, BLOCK)
        tb = jnp.pad(t, widths).reshape(t.shape[:axis] + (nb + 2, BLOCK) + t.shape[axis + 1:])
        parts = [lax.slice_in_dim(tb, s, s + nb, axis=axis) for s in range(3)]
        return jnp.concatenate(parts, axis=axis + 1)

    kn, vn = band(kp, 1), band(vp, 1)
    kpos, kreal = band(lpos, 0), band(is_real, 0)
    qpos = lpos.reshape(nb, BLOCK)
    visible = kreal[:, None, :] & (jnp.abs(qpos[:, :, None] - kpos[:, None, :]) <= WINDOW)

    qb = qp.reshape(B, nb, BLOCK, ATTN_KV_HEADS, ATTN_GROUP, HEAD_DIM)
    km, vm = k[:, :N_META], v[:, :N_META]
    scale = HEAD_DIM ** -0.5
    s_meta = jnp.einsum('bnqgrd,bmgd->bngrqm', qb, km).astype(jnp.float32) * scale
    s_band = jnp.einsum('bnqgrd,bnkgd->bngrqk', qb, kn).astype(jnp.float32) * scale
    s_band = jnp.where(visible[None, :, None, None], s_band, NEG_INF)
    sink = jnp.broadcast_to(sinks.astype(jnp.float32).reshape(1, 1, ATTN_KV_HEADS, ATTN_GROUP, 1, 1),
                            s_meta.shape[:-1] + (1,))
    p = jax.nn.softmax(jnp.concatenate([s_meta, s_band, sink], axis=-1), axis=-1)
    p_meta = p[..., :N_META].astype(v.dtype)
    p_band = p[..., N_META:N_META + 3 * BLOCK].astype(v.dtype)
    o = (jnp.einsum('bngrqm,bmgd->bnqgrd', p_meta, vm)
         + jnp.einsum('bngrqk,bnkgd->bnqgrd', p_band, vn))
    return from_padded(o.reshape(B, lp, Q_DIM), BLOCK)


def _linear_recurrence_op(e1, e2):
    a1, b1 = e1
    a2, b2 = e2
    return a1 * a2, a2 * b1 + b2


def s5_mixer(u, lam_re, lam_im, log_step, b_re, b_im, c_re, c_im, d_skip, w_glu, b_glu):
    f32 = jnp.float32
    B, L, _ = u.shape
    uf = u.astype(f32)
    ug = uf.reshape(B, L, S5_GROUPS, S5_GROUP)
    y = uf * d_skip.astype(f32)
    for direction in range(2):
        lam = lax.complex(jnp.minimum(lam_re[direction].astype(f32), -1e-4), lam_im[direction].astype(f32))
        step = jnp.exp(log_step[direction].astype(f32))[:, None]
        a_bar = jnp.exp(lam * step)
        b_bar = ((a_bar - 1.0) / lam)[..., None] * lax.complex(b_re[direction].astype(f32),
                                                                  b_im[direction].astype(f32))
        bu = lax.complex(jnp.einsum('blgc,gpc->blgp', ug, b_bar.real),
                         jnp.einsum('blgc,gpc->blgp', ug, b_bar.imag))
        _, xs = lax.associative_scan(_linear_recurrence_op, (jnp.broadcast_to(a_bar, bu.shape), bu),
                                     reverse=(direction == 1), axis=1)
        y_dir = (jnp.einsum('blgp,gcp->blgc', xs.real, c_re[direction].astype(f32))
                 - jnp.einsum('blgp,gcp->blgc', xs.imag, c_im[direction].astype(f32)))
        y = y + y_dir.reshape(B, L, S5_WIDTH)
    z = jax.nn.gelu(y)
    out = z * jax.nn.sigmoid(z @ w_glu.astype(f32) + b_glu.astype(f32))
    return out.astype(u.dtype)


def chunk_recurrence(q, k, v, g):
    B, lp = q.shape[:2]
    n = lp // HG_CHUNK

    def to_chunks(t):
        return t.reshape(B, n, HG_CHUNK, HG_HEADS, t.shape[-1]).transpose(1, 0, 3, 2, 4)

    tri = jnp.tril(jnp.ones((HG_CHUNK, HG_CHUNK), dtype=bool))[:, :, None]

    def step(state, chunk):
        qc, kc, vc, gc = chunk
        b = jnp.cumsum(gc, axis=2)
        rel = jnp.where(tri, b[:, :, :, None, :] - b[:, :, None, :, :], -jnp.inf)
        scores = jnp.sum(qc[:, :, :, None, :] * kc[:, :, None, :, :] * jnp.exp(rel), axis=-1)
        out = (jnp.einsum('bhts,bhsv->bhtv', scores, vc)
               + jnp.einsum('bhtk,bhkv->bhtv', qc * jnp.exp(b), state))
        b_end = b[:, :, -1:, :]
        state = (jnp.exp(b_end[:, :, 0, :, None]) * state
                 + jnp.einsum('bhsk,bhsv->bhkv', kc * jnp.exp(b_end - b), vc))
        return state, out

    state0 = jnp.zeros((B, HG_HEADS, HG_DK, HG_DV), q.dtype)
    _, out = lax.scan(step, state0, (to_chunks(q), to_chunks(k), to_chunks(v), to_chunks(g)))
    return out.transpose(1, 0, 3, 2, 4).reshape(B, lp, HG_HEADS, HG_DV)


def hgrn2_mixer(q, i_in, f_fw, f_bw, gate, lower_bound, norm_g):
    f32 = jnp.float32
    B, L, _ = q.shape
    lb = lower_bound.astype(f32).reshape(HG_HEADS, HG_DK)

    def heads(t):
        return t.astype(f32).reshape(B, L, HG_HEADS, -1)

    def forget(fz):
        f = lb + (1.0 - lb) * jax.nn.sigmoid(heads(fz))
        return jnp.log(f), 1.0 - f

    qh, vh = heads(q), heads(i_in)
    g1, k1 = forget(f_fw)
    g2, k2 = forget(f_bw)
    pad = lambda t: to_padded(t, HG_CHUNK)
    rev = lambda t: jnp.flip(t, axis=1)
    qp, vp = pad(qh), pad(vh)
    o_fw = chunk_recurrence(qp, pad(k1), vp, pad(g1))
    o_bw = rev(chunk_recurrence(rev(qp), rev(pad(k2)), rev(vp), rev(pad(g2))))
    o = from_padded(o_fw + o_bw, HG_CHUNK)
    o = o * lax.rsqrt(jnp.mean(o * o, axis=-1, keepdims=True) + RMS_EPS)
    o = o.reshape(B, L, HG_HEADS * HG_DV) * norm_g.astype(f32)
    return (o * jax.nn.sigmoid(gate.astype(f32))).astype(q.dtype)


def hier_moe(x, w_group, b_group, w_expert, b_expert, w_gate_up, w_down):
    f32 = jnp.float32
    B, L, D = x.shape
    T = B * L
    xt = x.reshape(T, D)
    xf = xt.astype(f32)
    g_logits = xf @ w_group.astype(f32) + b_group.astype(f32)
    grp = jnp.argmax(g_logits, axis=-1)
    p_grp = jnp.take_along_axis(jax.nn.softmax(g_logits, axis=-1), grp[:, None], axis=-1)
    e_logits = (xf @ w_expert.astype(f32) + b_expert.astype(f32)).reshape(T, N_GROUPS, EXPERTS_PER_GROUP)
    e_logits = jnp.take_along_axis(e_logits, grp[:, None, None], axis=1)[:, 0]
    top_v, top_i = lax.top_k(e_logits, TOP_K)
    weights = p_grp * jax.nn.softmax(top_v, axis=-1)
    expert = grp[:, None] * EXPERTS_PER_GROUP + top_i

    M = T * TOP_K
    flat_e = expert.reshape(M)
    flat_tok = jnp.repeat(jnp.arange(T), TOP_K)
    flat_w = weights.reshape(M)
    order = jnp.argsort(flat_e)
    e_sorted, tok_sorted, w_sorted = flat_e[order], flat_tok[order], flat_w[order]
    counts = jnp.bincount(flat_e, length=N_EXPERTS)
    padded = (counts + MOE_BLOCK - 1) // MOE_BLOCK * MOE_BLOCK
    start = jnp.cumsum(counts) - counts
    pend = jnp.cumsum(padded)
    pstart = pend - padded
    dest = pstart[e_sorted] + (jnp.arange(M) - start[e_sorted])
    n_blocks = -(-(M + N_EXPERTS * (MOE_BLOCK - 1)) // MOE_BLOCK)
    rows = n_blocks * MOE_BLOCK
    buf = jnp.zeros((rows, D), xt.dtype).at[dest].set(xt[tok_sorted])
    block_expert = jnp.minimum(jnp.searchsorted(pend, jnp.arange(n_blocks) * MOE_BLOCK, side='right'),
                               N_EXPERTS - 1)

    def expert_block(args):
        xb, e = args
        h = xb @ w_gate_up[e]
        return (jax.nn.silu(h[:, :D_EXPERT]) * h[:, D_EXPERT:]) @ w_down[e]

    ybuf = lax.map(expert_block, (buf.reshape(n_blocks, MOE_BLOCK, D), block_expert)).reshape(rows, D)
    y = ybuf[dest].astype(f32) * w_sorted[:, None]
    out = jnp.zeros((T, D), f32).at[tok_sorted].add(y)
    return out.reshape(B, L, D).astype(x.dtype)


def setup_inputs(seed: int = 0) -> dict:
    key = jax.random.key(seed)
    keys = iter(jax.random.split(key, 40))
    f32 = jnp.float32

    def nrm(shape, scale):
        return jax.random.normal(next(keys), shape, f32) * scale

    n_state = jnp.arange(S5_STATE, dtype=f32)
    s5_shape = (N_AB_LAYERS, 2, S5_GROUPS, S5_STATE)
    return {
        'x': nrm((BATCH, SEQ, D_MODEL), 1.0),
        'meta_tokens': nrm((N_META, D_MODEL), 1.0),
        'w_in_ab': nrm((N_AB_LAYERS, D_MODEL, AB_IN_DIM), D_MODEL ** -0.5),
        'w_out_ab': nrm((N_AB_LAYERS, AB_OUT_DIM, D_MODEL), AB_OUT_DIM ** -0.5 * DN_BETA),
        'attn_sinks': nrm((N_AB_LAYERS, ATTN_HEADS), 1.0),
        's5_lam_re': -0.5 + nrm(s5_shape, 0.01),
        's5_lam_im': math.pi * n_state + nrm(s5_shape, 0.01),
        's5_log_step': jax.random.uniform(next(keys), (N_AB_LAYERS, 2, S5_GROUPS), f32,
                                          math.log(1e-3), math.log(1e-1)),
        's5_b_re': nrm((N_AB_LAYERS, 2, S5_GROUPS, S5_STATE, S5_GROUP), (2 * S5_GROUP) ** -0.5),
        's5_b_im': nrm((N_AB_LAYERS, 2, S5_GROUPS, S5_STATE, S5_GROUP), (2 * S5_GROUP) ** -0.5),
        's5_c_re': nrm((N_AB_LAYERS, 2, S5_GROUPS, S5_GROUP, S5_STATE), S5_STATE ** -0.5),
        's5_c_im': nrm((N_AB_LAYERS, 2, S5_GROUPS, S5_GROUP, S5_STATE), S5_STATE ** -0.5),
        's5_d': nrm((N_AB_LAYERS, S5_WIDTH), 1.0),
        's5_w_glu': nrm((N_AB_LAYERS, S5_WIDTH, S5_WIDTH), S5_WIDTH ** -0.5),
        's5_b_glu': nrm((N_AB_LAYERS, S5_WIDTH), 0.01),
        'w_in_c': nrm((N_C_LAYERS, D_MODEL, C_IN_DIM), D_MODEL ** -0.5),
        'w_out_c': nrm((N_C_LAYERS, HG_HEADS * HG_DV, D_MODEL), (HG_HEADS * HG_DV) ** -0.5 * DN_BETA),
        'hgrn_lb_logits': nrm((DEPTH, HG_HEADS * HG_DK), 0.1),
        'hgrn_norm_g': 1.0 + nrm((N_C_LAYERS, HG_HEADS * HG_DV), 0.02),
        'ln_mix_g': 1.0 + nrm((DEPTH, D_MODEL), 0.02),
        'ln_mix_b': nrm((DEPTH, D_MODEL), 0.02),
        'ln_ffn_g': 1.0 + nrm((DEPTH, D_MODEL), 0.02),
        'ln_ffn_b': nrm((DEPTH, D_MODEL), 0.02),
        'moe_w_group': nrm((DEPTH, D_MODEL, N_GROUPS), D_MODEL ** -0.5),
        'moe_b_group': nrm((DEPTH, N_GROUPS), 0.01),
        'moe_w_expert': nrm((DEPTH, D_MODEL, N_EXPERTS), D_MODEL ** -0.5),
        'moe_b_expert': nrm((DEPTH, N_EXPERTS), 0.01),
        'moe_w_gate_up': nrm((DEPTH, N_EXPERTS, D_MODEL, 2 * D_EXPERT), D_MODEL ** -0.5),
        'moe_w_down': nrm((DEPTH, N_EXPERTS, D_EXPERT, D_MODEL), D_EXPERT ** -0.5 * DN_BETA),
    }


def reference(x, meta_tokens, w_in_ab, w_out_ab, attn_sinks, s5_lam_re, s5_lam_im, s5_log_step,
              s5_b_re, s5_b_im, s5_c_re, s5_c_im, s5_d, s5_w_glu, s5_b_glu, w_in_c, w_out_c,
              hgrn_lb_logits, hgrn_norm_g, ln_mix_g, ln_mix_b, ln_ffn_g, ln_ffn_b,
              moe_w_group, moe_b_group, moe_w_expert, moe_b_expert, moe_w_gate_up, moe_w_down):
    B = x.shape[0]
    meta = jnp.broadcast_to(meta_tokens.astype(x.dtype)[None], (B, N_META, D_MODEL))
    h = jnp.concatenate([meta, x], axis=1)
    L = h.shape[1]
    pos = jnp.arange(L)
    lb_probs = jax.nn.softmax(hgrn_lb_logits.astype(jnp.float32), axis=0)
    lb_table = jnp.cumsum(lb_probs, axis=0) - lb_probs[0]
    for layer in range(DEPTH):
        j = layer // 2
        if layer % 2 == 0:
            proj = h @ w_in_ab[j]
            q, k, v, u = jnp.split(proj, [Q_DIM, Q_DIM + KV_DIM, Q_DIM + 2 * KV_DIM], axis=-1)
            q = rope(q.reshape(B, L, ATTN_HEADS, HEAD_DIM), pos)
            k = rope(k.reshape(B, L, ATTN_KV_HEADS, HEAD_DIM), pos)
            v = v.reshape(B, L, ATTN_KV_HEADS, HEAD_DIM)
            a_out = window_attention(q, k, v, attn_sinks[j])
            s_out = s5_mixer(u, s5_lam_re[j], s5_lam_im[j], s5_log_step[j], s5_b_re[j], s5_b_im[j],
                             s5_c_re[j], s5_c_im[j], s5_d[j], s5_w_glu[j], s5_b_glu[j])
            mix = jnp.concatenate([a_out, s_out], axis=-1) @ w_out_ab[j]
        else:
            proj = h @ w_in_c[j]
            q, i_in, f_fw, f_bw, gate = jnp.split(proj, 5, axis=-1)
            mix = hgrn2_mixer(q, i_in, f_fw, f_bw, gate, lb_table[layer], hgrn_norm_g[j]) @ w_out_c[j]
        h = layer_norm(DN_ALPHA * h + mix, ln_mix_g[layer], ln_mix_b[layer])
        ffn = hier_moe(h, moe_w_group[layer], moe_b_group[layer], moe_w_expert[layer],
                       moe_b_expert[layer], moe_w_gate_up[layer], moe_w_down[layer])
        h = layer_norm(DN_ALPHA * h + ffn, ln_ffn_g[layer], ln_ffn_b[layer])
    return h[:, N_META:]
```

```python
from contextlib import ExitStack
import math
import numpy as np
import concourse.bass as bass
import concourse.mybir as mybir
from concourse.bass_utils import run_bass_kernel_spmd


F32 = mybir.dt.float32
BF16 = mybir.dt.bfloat16
I32 = mybir.dt.int32
AF = mybir.ActivationFunctionType
ALU = mybir.AluOpType
AX = mybir.AxisListType


class Prog:
    ENG = ("pe", "dve", "act", "pool", "sp")

    def __init__(self, nc, n_dsem=20):
        self.nc = nc
        self.E = {"pe": nc.tensor, "dve": nc.vector, "act": nc.scalar,
                  "pool": nc.gpsimd, "sp": nc.sync}
        self.csem = {e: nc.alloc_semaphore("cs_" + e) for e in self.ENG if e != "sp"}
        self.ccnt = {e: 0 for e in self.csem}
        self.dsem = [nc.alloc_semaphore("ds%d" % i) for i in range(n_dsem)]
        self.dcnt = [0] * n_dsem
        self.dnext = 0
        self.lastw = {}
        self.readers = {}
        self.known = {e: {} for e in self.ENG}
        self.n_inst = 0
        self.n_wait = 0
        self.bregs = {}

    def bound_reg(self, val):
        if val not in self.bregs:
            r = self.nc.gpsimd.alloc_register("bnd%d" % val)
            self.nc.gpsimd.reg_mov(r, val)
            self.bregs[val] = r
        return self.bregs[val]

    def _sem(self, sk):
        return self.csem[sk] if isinstance(sk, str) else self.dsem[sk]

    def _wait(self, eng, need):
        kn = self.known[eng]
        for sk, val in need.items():
            if kn.get(sk, 0) >= val:
                continue
            if sk == eng and eng == "pe":
                continue
            self.E[eng].wait_ge(self._sem(sk), val)
            self.n_wait += 1
            kn[sk] = val

    def emit(self, eng, fn, reads=(), writes=(), dma=False):
        need = {}

        def add(ev):
            sk, val = ev
            if need.get(sk, 0) < val:
                need[sk] = val

        for k in reads:
            if k in self.lastw:
                add(self.lastw[k])
        for k in writes:
            if k in self.lastw:
                add(self.lastw[k])
            for sk, val in self.readers.get(k, {}).items():
                add((sk, val))
        if dma:
            s = self.dnext
            self.dnext = (self.dnext + 1) % len(self.dsem)
            if self.dcnt[s] > 0:
                add((s, self.dcnt[s]))
            self._wait(eng, need)
            self.dcnt[s] += 16
            ev = (s, self.dcnt[s])
            fn(self.E[eng]).then_inc(self.dsem[s], 16)
        else:
            self._wait(eng, need)
            self.ccnt[eng] += 1
            ev = (eng, self.ccnt[eng])
            fn(self.E[eng]).then_inc(self.csem[eng], 1)
        self.n_inst += 1
        for k in writes:
            self.lastw[k] = ev
            self.readers[k] = {}
        for k in reads:
            if k in writes:
                continue
            r = self.readers.setdefault(k, {})
            if r.get(ev[0], 0) < ev[1]:
                r[ev[0]] = ev[1]
        return ev

    def dma(self, out, in_, reads=(), writes=(), eng="sp", **kw):
        return self.emit(eng, lambda e: e.dma_start(out=out, in_=in_, **kw),
                         reads=reads, writes=writes, dma=True)

    def collective(self, kind, ins, outs, reads=(), writes=(), n=8, bg=False):
        if bg:
            need = {}
            for k in list(reads) + list(writes):
                if k in self.lastw:
                    sk, val = self.lastw[k]
                    need[sk] = max(need.get(sk, 0), val)
            self._wait("pool", need)
            key = "ccbg%d" % len([k for k in self.csem if k.startswith("ccbg")])
            self.csem[key] = self.nc.alloc_semaphore("cs_" + key)
            self.nc.gpsimd.collective_compute(kind, ALU.bypass, replica_groups=[list(range(n))],
                                              ins=[a.opt() for a in ins], outs=[a.opt() for a in outs]).then_inc(self.csem[key], 1)
            for k in writes:
                self.lastw[k] = (key, 1)
                self.readers[k] = {}
            return (key, 1)
        if "cc" not in self.csem:
            self.csem["cc"] = self.nc.alloc_semaphore("cs_cc")
            self.cccnt = 0
        need = {}
        for k in reads:
            if k in self.lastw:
                sk, val = self.lastw[k]
                need[sk] = max(need.get(sk, 0), val)
        for k in writes:
            if k in self.lastw:
                sk, val = self.lastw[k]
                need[sk] = max(need.get(sk, 0), val)
            for sk, val in self.readers.get(k, {}).items():
                need[sk] = max(need.get(sk, 0), val)
        if self.cccnt:
            need["cc"] = max(need.get("cc", 0), self.cccnt)
        self._wait("pool", need)
        self.cccnt += 1
        ev = ("cc", self.cccnt)
        self.nc.gpsimd.collective_compute(kind, ALU.bypass, replica_groups=[list(range(n))],
                                          ins=[a.opt() for a in ins], outs=[a.opt() for a in outs]).then_inc(self.csem["cc"], 1)
        for k in writes:
            self.lastw[k] = ev
            self.readers[k] = {}
        for k in reads:
            r = self.readers.setdefault(k, {})
            r["cc"] = max(r.get("cc", 0), self.cccnt)
        return ev

    def barrier(self):
        need = {}
        for e, c in self.ccnt.items():
            if c:
                need[e] = c
        for i, c in enumerate(self.dcnt):
            if c:
                need[i] = c
        if "cc" in self.csem and self.cccnt:
            need["cc"] = self.cccnt
        for e in self.ENG:
            n2 = {k: v for k, v in need.items() if k != e or e == "sp"}
            if e in self.ccnt and self.ccnt[e]:
                n2[e] = self.ccnt[e]
            kn = self.known[e]
            for sk, val in n2.items():
                if kn.get(sk, 0) >= val:
                    continue
                self.E[e].wait_ge(self._sem(sk), val)
                kn[sk] = val
        keep = {k: v for k, v in self.lastw.items() if isinstance(v[0], str) and v[0].startswith("ccbg")}
        self.lastw = keep
        self.readers = {}


_uniq = [0]


def uniq(name):
    _uniq[0] += 1
    return "%s_%d" % (name, _uniq[0])


T = 2050
TH = 2432
NKT = 19
D = 2048
KT = 16


def qtiles():
    r = [(i * 128, 128) for i in range(16)]
    r.append((2048, 2))
    return r


def stage_inproj_ab(P, hT0, metaT, w_in, ropeC, ropeS, ropeCm, ropeSm, perm,
                    qT_d, kT_d, v_d, uT_d, kmT_d, vm_d):
    nc = P.nc
    with ExitStack() as es:
        def sb(name, shape, dt):
            return es.enter_context(nc.sbuf_tensor(uniq(name), shape, dt))

        def ps(name, shape, dt=F32):
            return es.enter_context(nc.psum_tensor(uniq(name), shape, dt))
        wb = sb("wb", [128, KT, 2560], BF16)
        xb = [sb("xb%d" % i, [128, KT, 512], BF16) for i in range(2)]
        cosb = sb("cosb", [128, TH], F32)
        sinb = sb("sinb", [128, TH], F32)
        cosm = sb("cosm", [128, 16], F32)
        sinm = sb("sinm", [128, 16], F32)
        permb = sb("permb", [128, 128], F32)
        qf = [sb("qf%d" % i, [128, 512], F32) for i in range(2)]
        t1 = [sb("t1%d" % i, [128, 512], F32) for i in range(2)]
        ob = [sb("ob%d" % i, [128, 512], BF16) for i in range(2)]
        vb = [sb("vb%d" % i, [128, 256], BF16) for i in range(2)]
        pp = [ps("pp%d" % i, [128, 512]) for i in range(2)]
        pr = [ps("pr%d" % i, [128, 512]) for i in range(2)]
        pv = [ps("pv%d" % i, [128, 256]) for i in range(2)]

        for kt in range(KT):
            for c0 in range(0, 2560, 1280):
                P.dma(wb[:, kt, c0:c0 + 1280], w_in[kt * 128:(kt + 1) * 128, c0:c0 + 1280],
                      writes=[("wb", kt, c0)], eng="pool")
        wkeys = [("wb", kt, c0) for kt in range(KT) for c0 in (0, 1280)]
        P.dma(cosb[:], ropeC[:, :], writes=["cosb"])
        P.dma(sinb[:], ropeS[:, :], writes=["sinb"])
        P.dma(cosm[:], ropeCm[:, :], writes=["cosm"])
        P.dma(sinm[:], ropeSm[:, :], writes=["sinm"])
        P.dma(permb[:], perm[:, :], writes=["permb"])

        cnt = {"pp": 0, "ob": 0, "vb": 0}

        def proj_rope(xk, xt, n, col0, cosap, sinap, dst):
            i = cnt["pp"] % 2
            cnt["pp"] += 1
            for kt in range(KT):
                P.emit("pe", lambda e, kt=kt: e.matmul(pp[i][:, :n], lhsT=wb[:, kt, col0:col0 + 128],
                                                       rhs=xt[:, kt, :n], start=(kt == 0), stop=(kt == KT - 1)),
                       reads=wkeys + [xk], writes=[("pp", i)])
            P.emit("act", lambda e: e.activation(out=qf[i][:, :n], in_=pp[i][:, :n], func=AF.Copy),
                   reads=[("pp", i)], writes=[("qf", i)])
            P.emit("pe", lambda e: e.matmul(pr[i][:, :n], lhsT=permb[:], rhs=qf[i][:, :n], start=True, stop=True),
                   reads=["permb", ("qf", i)], writes=[("pr", i)])
            P.emit("dve", lambda e: e.tensor_tensor(out=t1[i][:, :n], in0=qf[i][:, :n], in1=cosap, op=ALU.mult),
                   reads=[("qf", i), "cosb", "cosm"], writes=[("t1", i)])
            P.emit("dve", lambda e: e.tensor_tensor(out=qf[i][:, :n], in0=pr[i][:, :n], in1=sinap, op=ALU.mult),
                   reads=[("pr", i), "sinb", "sinm"], writes=[("qf", i)])
            P.emit("dve", lambda e: e.tensor_tensor(out=ob[i][:, :n], in0=t1[i][:, :n], in1=qf[i][:, :n], op=ALU.add),
                   reads=[("t1", i), ("qf", i)], writes=[("ob", i)])
            P.dma(dst, ob[i][:, :n], reads=[("ob", i)], writes=["dram_qk"])

        def proj_plain(xk, xt, n, col0, dst):
            i = cnt["pp"] % 2
            cnt["pp"] += 1
            for kt in range(KT):
                P.emit("pe", lambda e, kt=kt: e.matmul(pp[i][:, :n], lhsT=wb[:, kt, col0:col0 + 128],
                                                       rhs=xt[:, kt, :n], start=(kt == 0), stop=(kt == KT - 1)),
                       reads=wkeys + [xk], writes=[("pp", i)])
            P.emit("act", lambda e: e.activation(out=ob[i][:, :n], in_=pp[i][:, :n], func=AF.Copy),
                   reads=[("pp", i)], writes=[("ob", i)])
            P.dma(dst, ob[i][:, :n], reads=[("ob", i)], writes=["dram_u"])

        def proj_v(xk, xt, t0, dst):
            i = cnt["vb"] % 2
            cnt["vb"] += 1
            for kt in range(KT):
                P.emit("pe", lambda e, kt=kt: e.matmul(pv[i][:, :], lhsT=xt[:, kt, t0:t0 + 128],
                                                       rhs=wb[:, kt, 1280:1536], start=(kt == 0), stop=(kt == KT - 1)),
                       reads=wkeys + [xk], writes=[("pv", i)])
            P.emit("act", lambda e: e.activation(out=vb[i][:, :], in_=pv[i][:, :], func=AF.Copy),
                   reads=[("pv", i)], writes=[("vb", i)])
            P.dma(dst, vb[i][:, :], reads=[("vb", i)], writes=["dram_v"])

        blocks = [(b * 512, min(512, TH - b * 512)) for b in range((TH + 511) // 512)]
        for bi, (c0, n) in enumerate(blocks):
            x = xb[bi % 2]
            xk = ("xb", bi % 2)
            for kt in range(KT):
                P.dma(x[:, kt, :n], hT0[kt * 128:(kt + 1) * 128, c0:c0 + n], writes=[xk], eng="pool")
            for h in range(8):
                proj_rope(xk, x, n, h * 128, cosb[:, c0:c0 + n], sinb[:, c0:c0 + n], qT_d[h, :, c0:c0 + n])
            for g in range(2):
                proj_rope(xk, x, n, 1024 + g * 128, cosb[:, c0:c0 + n], sinb[:, c0:c0 + n], kT_d[g, :, c0:c0 + n])
            for t0 in range(0, n, 128):
                proj_v(xk, x, t0, v_d[c0 + t0:c0 + t0 + 128, :])
            for j in range(8):
                proj_plain(xk, x, n, 1536 + j * 128, uT_d[j * 128:(j + 1) * 128, c0:c0 + n])
        x = xb[len(blocks) % 2]
        xk = ("xb", len(blocks) % 2)
        for kt in range(KT):
            P.dma(x[:, kt, :16], metaT[kt * 128:(kt + 1) * 128, :], writes=[xk], eng="pool")
        for g in range(2):
            proj_rope(xk, x, 16, 1024 + g * 128, cosm[:, :], sinm[:, :], kmT_d[g, :, :])
        i = cnt["vb"] % 2
        for kt in range(KT):
            P.emit("pe", lambda e, kt=kt: e.matmul(pv[i][:16, :], lhsT=x[:, kt, 0:16],
                                                   rhs=wb[:, kt, 1280:1536], start=(kt == 0), stop=(kt == KT - 1)),
                   reads=wkeys + [xk], writes=[("pv", i)])
        P.emit("act", lambda e: e.activation(out=vb[i][:16, :], in_=pv[i][:16, :], func=AF.Copy),
               reads=[("pv", i)], writes=[("vb", i)])
        P.dma(vm_d[:, :], vb[i][:16, :], reads=[("vb", i)], writes=["dram_v"])
        P.barrier()


def stage_attn(P, qT_d, kT_d, v_d, kmT_d, vm_d, kvalid, maskL, maskR, sinks, aT_d):
    nc = P.nc
    SC = 128 ** -0.5
    with ExitStack() as es:
        def sb(name, shape, dt):
            return es.enter_context(nc.sbuf_tensor(uniq(name), shape, dt))

        def ps(name, shape, dt=F32):
            return es.enter_context(nc.psum_tensor(uniq(name), shape, dt))
        q = sb("q", [128, 8, TH], BF16)
        k = sb("k", [128, 2, TH], BF16)
        v = sb("v", [128, NKT, 256], BF16)
        km = sb("km", [128, 2, 16], BF16)
        vm = sb("vm", [16, 256], BF16)
        kv = sb("kv", [128, NKT], F32)
        mL = sb("mL", [128, 128], F32)
        mR = sb("mR", [128, 128], F32)
        snk = sb("snk", [128, 8], F32)
        esnk = sb("esnk", [128, 8], F32)
        ones = sb("ones", [128, 128], BF16)
        ex = [sb("ex%d" % i, [128, 512], F32) for i in range(3)]
        eb = [sb("eb%d" % i, [128, 512], BF16) for i in range(3)]
        ebm = sb("ebm", [16, 512], BF16)
        den = sb("den", [128, 512], F32)
        ao = [sb("ao%d" % i, [128, 512], BF16) for i in range(2)]
        psc = [ps("psc%d" % i, [128, 512]) for i in range(3)]
        pscm = ps("pscm", [16, 512])
        po = ps("po", [128, 512])
        pd = ps("pd", [128, 512])

        for h in range(8):
            P.dma(q[:, h, :], qT_d[h, :, :], writes=["q"])
        for g in range(2):
            P.dma(k[:, g, :], kT_d[g, :, :], writes=["k"])
            P.dma(km[:, g, :], kmT_d[g, :, :], writes=["km"])
        P.dma(v[:], v_d.rearrange("(j p) c -> p j c", p=128), writes=["v"])
        P.dma(vm[:], vm_d[:, :], writes=["vm"])
        P.dma(kv[:], kvalid[:, :], writes=["kv"])
        P.dma(mL[:], maskL[:, :], writes=["mL"])
        P.dma(mR[:], maskR[:, :], writes=["mR"])
        P.dma(snk[:], sinks[:, :], writes=["snk"])
        P.emit("act", lambda e: e.activation(out=esnk[:], in_=snk[:], func=AF.Exp), reads=["snk"], writes=["esnk"])
        P.emit("pool", lambda e: e.memset(ones[:], 1.0), writes=["ones"])

        it = 0
        for ti, (q0, s) in enumerate(qtiles()):
            for g in range(2):
                n = 4 * s
                qcol = 128 + q0
                for jj in range(3):
                    j = ti + jj
                    for hh in range(4):
                        P.emit("pe", lambda e, jj=jj, j=j, hh=hh: e.matmul(
                            psc[jj][:, hh * s:(hh + 1) * s], lhsT=k[:, g, j * 128:(j + 1) * 128],
                            rhs=q[:, 4 * g + hh, qcol:qcol + s], start=True, stop=True),
                            reads=["q", "k"], writes=[("psc", jj)])
                    P.emit("act", lambda e, jj=jj: e.activation(out=ex[jj][:, :n], in_=psc[jj][:, :n],
                                                                 func=AF.Exp, scale=SC),
                           reads=[("psc", jj)], writes=[("ex", jj)])
                    if jj == 1:
                        P.emit("dve", lambda e, jj=jj, j=j: e.tensor_scalar(
                            out=eb[jj][:, :n], in0=ex[jj][:, :n], scalar1=kv[:, j:j + 1], scalar2=None,
                            op0=ALU.mult), reads=[("ex", jj), "kv"], writes=[("eb", jj)])
                    else:
                        m = mL if jj == 0 else mR
                        for hh in range(4):
                            P.emit("dve", lambda e, jj=jj, j=j, hh=hh, m=m: e.scalar_tensor_tensor(
                                out=eb[jj][:, hh * s:(hh + 1) * s], in0=ex[jj][:, hh * s:(hh + 1) * s],
                                scalar=kv[:, j:j + 1], in1=m[:, :s], op0=ALU.mult, op1=ALU.mult),
                                reads=[("ex", jj), "kv", "mL", "mR"], writes=[("eb", jj)])
                for hh in range(4):
                    P.emit("pe", lambda e, hh=hh: e.matmul(
                        pscm[:, hh * s:(hh + 1) * s], lhsT=km[:, g, :],
                        rhs=q[:, 4 * g + hh, qcol:qcol + s], start=True, stop=True),
                        reads=["q", "km"], writes=["pscm"])
                P.emit("act", lambda e: e.activation(out=ebm[:, :n], in_=pscm[:, :n], func=AF.Exp, scale=SC),
                       reads=["pscm"], writes=["ebm"])
                for jj in range(3):
                    j = ti + jj
                    P.emit("pe", lambda e, jj=jj, j=j: e.matmul(
                        po[:, :n], lhsT=v[:, j, g * 128:(g + 1) * 128], rhs=eb[jj][:, :n],
                        start=(jj == 0), stop=False), reads=["v", ("eb", jj)], writes=["po"])
                P.emit("pe", lambda e: e.matmul(po[:, :n], lhsT=vm[:, g * 128:(g + 1) * 128], rhs=ebm[:, :n],
                                                start=False, stop=True), reads=["vm", "ebm"], writes=["po"])
                for jj in range(3):
                    P.emit("pe", lambda e, jj=jj: e.matmul(pd[:, :n], lhsT=ones[:, :], rhs=eb[jj][:, :n],
                                                            start=(jj == 0), stop=False),
                           reads=["ones", ("eb", jj)], writes=["pd"])
                P.emit("pe", lambda e: e.matmul(pd[:, :n], lhsT=ones[:16, :], rhs=ebm[:, :n],
                                                start=False, stop=True), reads=["ones", "ebm"], writes=["pd"])
                for hh in range(4):
                    P.emit("dve", lambda e, hh=hh: e.tensor_scalar(
                        out=den[:, hh * s:(hh + 1) * s], in0=pd[:, hh * s:(hh + 1) * s],
                        scalar1=esnk[:, 4 * g + hh:4 * g + hh + 1], scalar2=None, op0=ALU.add),
                        reads=["pd", "esnk"], writes=["den"])
                P.emit("dve", lambda e: e.reciprocal(out=den[:, :n], in_=den[:, :n]), reads=["den"], writes=["den"])
                a = ao[it % 2]
                ak = ("ao", it % 2)
                it += 1
                P.emit("dve", lambda e, a=a: e.tensor_tensor(out=a[:, :n], in0=po[:, :n], in1=den[:, :n], op=ALU.mult),
                       reads=["po", "den"], writes=[ak])
                for hh in range(4):
                    P.dma(aT_d[4 * g + hh, :, q0:q0 + s], a[:, hh * s:(hh + 1) * s], reads=[ak], writes=["dram_a"])
        P.barrier()


T = 2050
TH = 2432
TC = 64
TWO_PI = 2.0 * math.pi


def s5_chunks():
    r = [(i * TC, TC) for i in range(T // TC)]
    if T % TC:
        r.append((T - T % TC, T % TC))
    return r


def cexp_alloc(es, nc, tag, F):
    names = ["lr", "li", "st", "zr", "zi", "mag", "nf", "r", "m1", "r2", "ar", "ai", "wr", "wi"]
    tl = {n: es.enter_context(nc.sbuf_tensor(uniq(tag + n), [128, F], F32)) for n in names}
    tl["ni"] = es.enter_context(nc.sbuf_tensor(uniq(tag + "ni"), [128, F], I32))
    return tl


def cexp_consts(P, tl, tag, lam3):
    lr, li, st = tl["lr"], tl["li"], tl["st"]
    zr, zi, mag = tl["zr"], tl["zi"], tl["mag"]
    nf, r, m1, r2 = tl["nf"], tl["r"], tl["m1"], tl["r2"]
    ni = tl["ni"]
    ar, ai, wr, wi = tl["ar"], tl["ai"], tl["wr"], tl["wi"]
    k = lambda s: tag + s
    P.dma(lr[:], lam3[0], writes=[k("lr")])
    P.dma(li[:], lam3[1], writes=[k("li")])
    P.dma(st[:], lam3[2], writes=[k("st")])
    D = lambda fn, rd, wr_: P.emit("dve", fn, reads=[k(x) for x in rd], writes=[k(x) for x in wr_])
    A = lambda fn, rd, wr_: P.emit("act", fn, reads=[k(x) for x in rd], writes=[k(x) for x in wr_])
    D(lambda e: e.tensor_scalar(out=lr[:], in0=lr[:], scalar1=-1e-4, scalar2=None, op0=ALU.min), ["lr"], ["lr"])
    A(lambda e: e.activation(out=st[:], in_=st[:], func=AF.Exp), ["st"], ["st"])
    D(lambda e: e.tensor_tensor(out=zr[:], in0=lr[:], in1=st[:], op=ALU.mult), ["lr", "st"], ["zr"])
    D(lambda e: e.tensor_tensor(out=zi[:], in0=li[:], in1=st[:], op=ALU.mult), ["li", "st"], ["zi"])
    A(lambda e: e.activation(out=mag[:], in_=zr[:], func=AF.Exp), ["zr"], ["mag"])
    D(lambda e: e.tensor_scalar(out=nf[:], in0=zi[:], scalar1=1.0 / TWO_PI, scalar2=None, op0=ALU.mult), ["zi"], ["nf"])
    D(lambda e: e.tensor_copy(out=ni[:], in_=nf[:]), ["nf"], ["ni"])
    D(lambda e: e.tensor_copy(out=nf[:], in_=ni[:]), ["ni"], ["nf"])
    D(lambda e: e.scalar_tensor_tensor(out=r[:], in0=nf[:], scalar=-TWO_PI, in1=zi[:], op0=ALU.mult, op1=ALU.add),
      ["nf", "zi"], ["r"])

    def fold(t):
        D(lambda e: e.tensor_single_scalar(out=m1[:], in_=t[:], scalar=math.pi, op=ALU.is_gt), ["r", "r2"], ["m1"])
        D(lambda e: e.scalar_tensor_tensor(out=t[:], in0=m1[:], scalar=-TWO_PI, in1=t[:], op0=ALU.mult, op1=ALU.add),
          ["m1", "r", "r2"], ["r", "r2"])
        D(lambda e: e.tensor_single_scalar(out=m1[:], in_=t[:], scalar=-math.pi, op=ALU.is_lt), ["r", "r2"], ["m1"])
        D(lambda e: e.scalar_tensor_tensor(out=t[:], in0=m1[:], scalar=TWO_PI, in1=t[:], op0=ALU.mult, op1=ALU.add),
          ["m1", "r", "r2"], ["r", "r2"])
    fold(r)
    D(lambda e: e.tensor_scalar(out=r2[:], in0=r[:], scalar1=math.pi / 2, scalar2=None, op0=ALU.add), ["r"], ["r2"])
    fold(r2)
    A(lambda e: e.activation(out=ai[:], in_=r[:], func=AF.Sin), ["r"], ["ai"])
    A(lambda e: e.activation(out=ar[:], in_=r2[:], func=AF.Sin), ["r2"], ["ar"])
    D(lambda e: e.tensor_tensor(out=ai[:], in0=ai[:], in1=mag[:], op=ALU.mult), ["ai", "mag"], ["ai"])
    D(lambda e: e.tensor_tensor(out=ar[:], in0=ar[:], in1=mag[:], op=ALU.mult), ["ar", "mag"], ["ar"])
    D(lambda e: e.tensor_tensor(out=zr[:], in0=lr[:], in1=lr[:], op=ALU.mult), ["lr"], ["zr"])
    D(lambda e: e.tensor_tensor(out=zi[:], in0=li[:], in1=li[:], op=ALU.mult), ["li"], ["zi"])
    D(lambda e: e.tensor_tensor(out=zr[:], in0=zr[:], in1=zi[:], op=ALU.add), ["zr", "zi"], ["zr"])
    D(lambda e: e.reciprocal(out=zr[:], in_=zr[:]), ["zr"], ["zr"])
    D(lambda e: e.tensor_scalar(out=nf[:], in0=ar[:], scalar1=-1.0, scalar2=None, op0=ALU.add), ["ar"], ["nf"])
    D(lambda e: e.tensor_tensor(out=wr[:], in0=nf[:], in1=lr[:], op=ALU.mult), ["nf", "lr"], ["wr"])
    D(lambda e: e.tensor_tensor(out=r[:], in0=ai[:], in1=li[:], op=ALU.mult), ["ai", "li"], ["r"])
    D(lambda e: e.tensor_tensor(out=wr[:], in0=wr[:], in1=r[:], op=ALU.add), ["wr", "r"], ["wr"])
    D(lambda e: e.tensor_tensor(out=wr[:], in0=wr[:], in1=zr[:], op=ALU.mult), ["wr", "zr"], ["wr"])
    D(lambda e: e.tensor_tensor(out=wi[:], in0=ai[:], in1=lr[:], op=ALU.mult), ["ai", "lr"], ["wi"])
    D(lambda e: e.tensor_tensor(out=r[:], in0=nf[:], in1=li[:], op=ALU.mult), ["nf", "li"], ["r"])
    D(lambda e: e.tensor_tensor(out=wi[:], in0=wi[:], in1=r[:], op=ALU.subtract), ["wi", "r"], ["wi"])
    D(lambda e: e.tensor_tensor(out=wi[:], in0=wi[:], in1=zr[:], op=ALU.mult), ["wi", "zr"], ["wi"])
    return ar, ai, wr, wi


def stage_s5_scan(P, uT_d, lamT, lamR, Bl, Cl, carry_in, end_d, yF_d, yB_d, full):
    nc = P.nc
    FB = 1024
    with ExitStack() as es:
        def sb(name, shape, dt):
            return es.enter_context(nc.sbuf_tensor(uniq(name), shape, dt))
        uT = sb("uT", [128, 8, T], BF16)
        Bb = sb("Bb", [128, 2, 2, 32, 128], BF16)
        Cb = sb("Cb", [128, 2, 2, 32, 128], BF16) if full else None
        A1 = sb("A1", [128, 2, 2, 32], F32)
        A2 = sb("A2", [128, 2, 2, 32], F32)
        S0 = sb("S0", [128, 2, 2, 32], F32)

        for m in range(8):
            P.dma(uT[:, m, :], uT_d[m * 128:(m + 1) * 128, 128:128 + T], writes=["uT"])

        with ExitStack() as es2:
            tls = cexp_alloc(es2, nc, "cs", 32)
            tlb = cexp_alloc(es2, nc, "cb", FB)
            ATr = es2.enter_context(nc.sbuf_tensor(uniq("ATr"), [128, 2, 32], F32))
            ATi = es2.enter_context(nc.sbuf_tensor(uniq("ATi"), [128, 2, 32], F32))
            for d in range(2):
                ar, ai, wr, wi = cexp_consts(P, tls, "cs", lamT[d])
                P.emit("dve", lambda e, d=d, ar=ar: e.tensor_copy(out=A1[:, d, 0, :], in_=ar[:]), reads=["csar"], writes=["A1"])
                P.emit("dve", lambda e, d=d, ar=ar: e.tensor_copy(out=A1[:, d, 1, :], in_=ar[:]), reads=["csar"], writes=["A1"])
                P.emit("dve", lambda e, d=d, ai=ai: e.tensor_scalar(out=A2[:, d, 0, :], in0=ai[:], scalar1=-1.0, scalar2=None,
                                                                    op0=ALU.mult), reads=["csai"], writes=["A2"])
                P.emit("dve", lambda e, d=d, ai=ai: e.tensor_copy(out=A2[:, d, 1, :], in_=ai[:]), reads=["csai"], writes=["A2"])
            if carry_in is not None:
                def sbt(name):
                    return es2.enter_context(nc.sbuf_tensor(uniq(name), [128, 2, 32], F32))
                pr_, pi_, qr_, qi_, t_, a2r, a2i = [sbt("pw%d" % i) for i in range(7)]
                ends_all_d, fm_d, bm_d = carry_in
                E = es2.enter_context(nc.sbuf_tensor(uniq("Ecar"), [128, 2, 8, 64], F32))
                mkc = es2.enter_context(nc.sbuf_tensor(uniq("mkc"), [128, 2, 8], F32))
                tre = es2.enter_context(nc.sbuf_tensor(uniq("tre"), [128, 32], F32))
                for d in range(2):
                    P.dma(E[:, d, :, :], ends_all_d[:, d].rearrange("c p f -> p c f"), writes=["Ecar"])
                P.dma(mkc[:, 0, :], fm_d[:, :], writes=["mkc"])
                P.dma(mkc[:, 1, :], bm_d[:, :], writes=["mkc"])
                kp = ["pw"]

                def cmul(orr, oi, xr, xi, yr, yi):
                    rd = kp + ["A1", "A2", "S0"]
                    P.emit("dve", lambda e: e.tensor_tensor(out=orr, in0=xr, in1=yr, op=ALU.mult), reads=rd, writes=kp)
                    P.emit("dve", lambda e: e.tensor_tensor(out=t_[:], in0=xi, in1=yi, op=ALU.mult), reads=rd, writes=kp)
                    P.emit("dve", lambda e: e.tensor_tensor(out=orr, in0=orr, in1=t_[:], op=ALU.subtract), reads=kp, writes=kp)
                    P.emit("dve", lambda e: e.tensor_tensor(out=oi, in0=xr, in1=yi, op=ALU.mult), reads=rd, writes=kp)
                    P.emit("dve", lambda e: e.tensor_tensor(out=t_[:], in0=xi, in1=yr, op=ALU.mult), reads=rd, writes=kp)
                    P.emit("dve", lambda e: e.tensor_tensor(out=oi, in0=oi, in1=t_[:], op=ALU.add), reads=kp, writes=kp)
                Ar = A1[:, :, 0, :]
                Ai = A2[:, :, 1, :]
                cmul(a2r[:], a2i[:], Ar, Ai, Ar, Ai)
                cur = (a2r, a2i)
                bufs = [(pr_, pi_), (qr_, qi_)]
                for i in range(10):
                    o = bufs[i % 2]
                    cmul(o[0][:], o[1][:], cur[0][:], cur[1][:], cur[0][:], cur[1][:])
                    cur = o
                cmul(ATr[:], ATi[:], cur[0][:], cur[1][:], a2r[:], a2i[:])
                sr, si_ = pr_, pi_
                P.emit("dve", lambda e: e.memset(S0[:], 0.0), writes=["S0", ("S0", 0), ("S0", 1)])
                Sr = S0[:, :, 0, :]
                Si = S0[:, :, 1, :]
                for kcar in range(8):
                    cmul(sr[:], si_[:], ATr[:], ATi[:], Sr, Si)
                    for d in range(2):
                        cc = kcar if d == 0 else 7 - kcar
                        m = mkc[:, d, cc:cc + 1]
                        for comp, src in ((0, sr), (1, si_)):
                            P.emit("dve", lambda e, d=d, cc=cc, comp=comp, src=src: e.tensor_tensor(
                                out=tre[:], in0=src[:, d, :], in1=E[:, d, cc, comp * 32:(comp + 1) * 32], op=ALU.add),
                                reads=kp + ["Ecar"], writes=["tre"])
                            P.emit("dve", lambda e, d=d, comp=comp: e.tensor_tensor(out=tre[:], in0=tre[:], in1=S0[:, d, comp, :], op=ALU.subtract),
                                   reads=["tre", "S0"], writes=["tre"])
                            P.emit("dve", lambda e, d=d, comp=comp, m=m: e.scalar_tensor_tensor(
                                out=S0[:, d, comp, :], in0=tre[:], scalar=m, in1=S0[:, d, comp, :], op0=ALU.mult, op1=ALU.add),
                                reads=["tre", "S0", "mkc"], writes=["S0", ("S0", 0), ("S0", 1)])
            else:
                P.emit("dve", lambda e: e.memset(S0[:], 0.0), writes=["S0", ("S0", 0), ("S0", 1)])
            bre = es2.enter_context(nc.sbuf_tensor(uniq("bre"), [128, FB], F32))
            bim = es2.enter_context(nc.sbuf_tensor(uniq("bim"), [128, FB], F32))
            tt = es2.enter_context(nc.sbuf_tensor(uniq("btt"), [128, FB], F32))
            t2 = es2.enter_context(nc.sbuf_tensor(uniq("bt2"), [128, FB], F32))
            for d in range(2):
                for blk in range(4096 // FB):
                    cs = slice(blk * FB, (blk + 1) * FB)
                    ar, ai, wr, wi = cexp_consts(P, tlb, "cb", lamR[d][:, :, cs])
                    P.dma(bre[:], Bl[d, 0].rearrange("p q j -> p (q j)")[:, cs], writes=["bre"])
                    P.dma(bim[:], Bl[d, 1].rearrange("p q j -> p (q j)")[:, cs], writes=["bim"])
                    ore = Bb[:, d, 0].rearrange("p q j -> p (q j)")[:, cs]
                    oim = Bb[:, d, 1].rearrange("p q j -> p (q j)")[:, cs]
                    P.emit("dve", lambda e, wi=wi: e.tensor_tensor(out=tt[:], in0=wi[:], in1=bim[:], op=ALU.mult),
                           reads=["cbwi", "bim"], writes=["btt"])
                    P.emit("dve", lambda e, wr=wr: e.tensor_tensor(out=t2[:], in0=wr[:], in1=bre[:], op=ALU.mult),
                           reads=["cbwr", "bre"], writes=["bt2"])
                    P.emit("dve", lambda e, ore=ore: e.tensor_tensor(out=ore, in0=t2[:], in1=tt[:], op=ALU.subtract),
                           reads=["bt2", "btt"], writes=["Bb"])
                    P.emit("dve", lambda e, wr=wr: e.tensor_tensor(out=tt[:], in0=wr[:], in1=bim[:], op=ALU.mult),
                           reads=["cbwr", "bim"], writes=["btt"])
                    P.emit("dve", lambda e, wi=wi: e.tensor_tensor(out=t2[:], in0=wi[:], in1=bre[:], op=ALU.mult),
                           reads=["cbwi", "bre"], writes=["bt2"])
                    P.emit("dve", lambda e, oim=oim: e.tensor_tensor(out=oim, in0=t2[:], in1=tt[:], op=ALU.add),
                           reads=["bt2", "btt"], writes=["Bb"])
                    if full:
                        P.dma(bre[:], Cl[d, 0].rearrange("p q j -> p (q j)")[:, cs], writes=["bre"])
                        P.dma(bim[:], Cl[d, 1].rearrange("p q j -> p (q j)")[:, cs], writes=["bim"])
                        P.emit("act", lambda e, d=d, cs=cs: e.activation(out=Cb[:, d, 0].rearrange("p q j -> p (q j)")[:, cs],
                                                                          in_=bre[:], func=AF.Copy), reads=["bre"], writes=["Cb"])
                        P.emit("act", lambda e, d=d, cs=cs: e.activation(out=Cb[:, d, 1].rearrange("p q j -> p (q j)")[:, cs],
                                                                          in_=bim[:], func=AF.Copy, scale=-1.0), reads=["bim"], writes=["Cb"])
            P.barrier()

        def ps(name, shape, dt=F32):
            return es.enter_context(nc.psum_tensor(uniq(name), shape, dt))
        X = sb("X", [128, 2, 2, 32, TC], F32)
        Xb = sb("Xb", [128, 2, 2, 32, TC], BF16) if full else None
        P1 = [sb("P1%d" % i, [128, 2, 2, 16], F32) for i in range(2)]
        P2 = [sb("P2%d" % i, [128, 2, 2, 16], F32) for i in range(2)]
        yst = sb("yst", [128, 2, 8, TC], F32) if full else None
        pb = [ps("pb%d" % i, [128, 512]) for i in range(4)]
        py = [ps("py%d" % i, [128, 512]) for i in range(2)] if full else None

        chunks = s5_chunks()
        pbi = 0
        for ci, (c0, n) in enumerate(chunks):
            f0 = c0
            b0 = T - c0 - n
            for d in range(2):
                t0 = f0 if d == 0 else b0
                for c in range(2):
                    for qg in range(4):
                        pt = pb[pbi % 4]
                        pk = ("pb", pbi % 4)
                        pbi += 1
                        for qq in range(8):
                            q = qg * 8 + qq
                            m = q // 4
                            P.emit("pe", lambda e, pt=pt, qq=qq, d=d, c=c, q=q, m=m, t0=t0: e.matmul(
                                pt[:, qq * n:(qq + 1) * n], lhsT=Bb[:, d, c, q, :], rhs=uT[:, m, t0:t0 + n],
                                start=True, stop=True), reads=["Bb", "uT"], writes=[pk])
                        src = pt[:, :8 * n].rearrange("p (q t) -> p q t", t=n)
                        if d == 0:
                            dst = X[:, d, c, qg * 8:(qg + 1) * 8, 0:n]
                        else:
                            dst = X[:, d, c, qg * 8:(qg + 1) * 8, n - 1::-1]
                        P.emit("act", lambda e, src=src, dst=dst: e.activation(out=dst, in_=src, func=AF.Copy),
                               reads=[pk], writes=[("X", qg // 2)])
            for t in range(n):
                hs = []
                for hh in range(2):
                    qs = slice(16 * hh, 16 * hh + 16)
                    Sp = S0[:, :, :, qs] if t == 0 else X[:, :, :, qs, t - 1]
                    Sps = S0[:, :, ::-1, qs] if t == 0 else X[:, :, ::-1, qs, t - 1]
                    hs.append((hh, qs, Sp, Sps))
                for (hh, qs, Sp, Sps) in hs:
                    P.emit("dve", lambda e, Sp=Sp, hh=hh, qs=qs: e.tensor_tensor(out=P1[hh][:], in0=A1[:, :, :, qs], in1=Sp, op=ALU.mult),
                           reads=["A1", ("X", hh), ("S0", hh)], writes=[("P1", hh)])
                for (hh, qs, Sp, Sps) in hs:
                    P.emit("dve", lambda e, Sps=Sps, hh=hh, qs=qs: e.tensor_tensor(out=P2[hh][:], in0=A2[:, :, :, qs], in1=Sps, op=ALU.mult),
                           reads=["A2", ("X", hh), ("S0", hh)], writes=[("P2", hh)])
                for (hh, qs, Sp, Sps) in hs:
                    P.emit("dve", lambda e, hh=hh: e.tensor_tensor(out=P1[hh][:], in0=P1[hh][:], in1=P2[hh][:], op=ALU.add),
                           reads=[("P1", hh), ("P2", hh)], writes=[("P1", hh)])
                for (hh, qs, Sp, Sps) in hs:
                    P.emit("dve", lambda e, t=t, hh=hh, qs=qs: e.tensor_tensor(out=X[:, :, :, qs, t], in0=X[:, :, :, qs, t], in1=P1[hh][:], op=ALU.add),
                           reads=[("X", hh), ("P1", hh)], writes=[("X", hh)])
            for hh in range(2):
                qs = slice(16 * hh, 16 * hh + 16)
                P.emit("dve", lambda e, n=n, qs=qs: e.tensor_copy(out=S0[:, :, :, qs], in_=X[:, :, :, qs, n - 1]),
                       reads=[("X", hh)], writes=[("S0", hh)])
            if full:
                P.emit("act", lambda e, n=n: e.activation(out=Xb[:, :, :, :, :n], in_=X[:, :, :, :, :n], func=AF.Copy),
                       reads=[("X", 0), ("X", 1)], writes=["Xb"])
                for d in range(2):
                    pyt = py[d]
                    for m in range(8):
                        idx = 0
                        for qq in range(4):
                            q = 4 * m + qq
                            for c in range(2):
                                P.emit("pe", lambda e, pyt=pyt, m=m, d=d, c=c, q=q, idx=idx: e.matmul(
                                    pyt[:, m * n:(m + 1) * n], lhsT=Cb[:, d, c, q, :], rhs=Xb[:, d, c, q, :n],
                                    start=(idx == 0), stop=(idx == 7)), reads=["Cb", "Xb"], writes=[("py", d)])
                                idx += 1
                    src = pyt[:, :8 * n].rearrange("p (m t) -> p m t", t=n)
                    if d == 0:
                        dst = yst[:, d, :, 0:n]
                    else:
                        dst = yst[:, d, :, n - 1::-1]
                    P.emit("dve", lambda e, src=src, dst=dst: e.tensor_copy(out=dst, in_=src),
                           reads=[("py", d)], writes=[("yst", d)])
                    t0 = f0 if d == 0 else b0
                    yd = yF_d if d == 0 else yB_d
                    P.dma(yd.rearrange("(m p) t -> p m t", p=128)[:, :, t0:t0 + n], yst[:, d, :, :n],
                          reads=[("yst", d)], writes=["y_dram"])
        for d in range(2):
            P.dma(end_d[d], S0[:, d].rearrange("p c q -> p (c q)"), reads=["S0", ("S0", 0), ("S0", 1)], writes=["end_dram"])
        P.barrier()


def stage_s5_glu(P, uT_d, yF_d, yB_d, dsk, wglu, bglu, sT_d):
    nc = P.nc
    with ExitStack() as es:
        def sb(name, shape, dt):
            return es.enter_context(nc.sbuf_tensor(uniq(name), shape, dt))

        def ps(name, shape, dt=F32):
            return es.enter_context(nc.psum_tensor(uniq(name), shape, dt))
        wg = sb("wg", [128, 8, 1024], BF16)
        dk = sb("dk", [128, 8], F32)
        bg = sb("bg", [128, 8], F32)
        ub = [sb("ub%d" % i, [128, 8, 512], BF16) for i in range(2)]
        yf = [sb("yf%d" % i, [128, 8, 512], F32) for i in range(2)]
        yb = [sb("yb%d" % i, [128, 8, 512], F32) for i in range(2)]
        zb = [sb("zb%d" % i, [128, 8, 512], BF16) for i in range(2)]
        sg = [sb("sg%d" % i, [128, 512], F32) for i in range(2)]
        ob = [sb("ob%d" % i, [128, 512], BF16) for i in range(2)]
        pg = [ps("pg%d" % i, [128, 512]) for i in range(2)]
        for kt in range(8):
            P.dma(wg[:, kt, :], wglu[kt * 128:(kt + 1) * 128, :], writes=["wg"], eng="pool")
        P.dma(dk[:], dsk[:, :], writes=["dk"])
        P.dma(bg[:], bglu[:, :], writes=["bg"])
        blocks = [(b * 512, min(512, T - b * 512)) for b in range((T + 511) // 512)]
        it = 0
        for bi, (c0, n) in enumerate(blocks):
            i = bi % 2
            P.dma(ub[i][:, :, :n], uT_d.rearrange("(m p) t -> p m t", p=128)[:, :, 128 + c0:128 + c0 + n], writes=[("ub", i)])
            P.dma(yf[i][:, :, :n], yF_d.rearrange("(m p) t -> p m t", p=128)[:, :, c0:c0 + n], writes=[("yf", i)])
            P.dma(yb[i][:, :, :n], yB_d.rearrange("(m p) t -> p m t", p=128)[:, :, c0:c0 + n], writes=[("yb", i)])
            for m in range(8):
                P.emit("dve", lambda e, m=m: e.scalar_tensor_tensor(out=yf[i][:, m, :n], in0=ub[i][:, m, :n], scalar=dk[:, m:m + 1],
                                                                    in1=yf[i][:, m, :n], op0=ALU.mult, op1=ALU.add),
                       reads=[("ub", i), ("yf", i), "dk"], writes=[("yf", i)])
            P.emit("dve", lambda e: e.tensor_tensor(out=yf[i][:, :, :n], in0=yf[i][:, :, :n], in1=yb[i][:, :, :n], op=ALU.add),
                   reads=[("yf", i), ("yb", i)], writes=[("yf", i)])
            P.emit("act", lambda e: e.activation(out=yf[i][:, :, :n], in_=yf[i][:, :, :n], func=AF.Gelu_apprx_tanh),
                   reads=[("yf", i)], writes=[("yf", i)])
            P.emit("pool", lambda e: e.tensor_copy(out=zb[i][:, :, :n], in_=yf[i][:, :, :n]), reads=[("yf", i)], writes=[("zb", i)])
            for j in range(8):
                k2 = it % 2
                it += 1
                for kt in range(8):
                    P.emit("pe", lambda e, kt=kt, j=j, k2=k2: e.matmul(pg[k2][:, :n], lhsT=wg[:, kt, j * 128:(j + 1) * 128],
                                                                        rhs=zb[i][:, kt, :n], start=(kt == 0), stop=(kt == 7)),
                           reads=["wg", ("zb", i)], writes=[("pg", k2)])
                P.emit("act", lambda e, j=j, k2=k2: e.activation(out=sg[k2][:, :n], in_=pg[k2][:, :n], func=AF.Sigmoid,
                                                                  bias=bg[:, j:j + 1]), reads=[("pg", k2), "bg"], writes=[("sg", k2)])
                P.emit("dve", lambda e, j=j, k2=k2: e.tensor_tensor(out=ob[k2][:, :n], in0=yf[i][:, j, :n], in1=sg[k2][:, :n], op=ALU.mult),
                       reads=[("yf", i), ("sg", k2)], writes=[("ob", k2)])
                P.dma(sT_d[j * 128:(j + 1) * 128, c0:c0 + n], ob[k2][:, :n], reads=[("ob", k2)], writes=["s_dram"])
        P.barrier()


T = 2050
D = 2048
ALPHA = 4.0 ** 0.25
LN_EPS = 1e-5
NBLK = 96
BIG = 1e30


def ttiles():
    r = [(i * 128, 128) for i in range(16)]
    r.append((2048, 2))
    return r


class LNCtx:
    def __init__(self, P, es, ident_d, g_rep_d, b_rep_d, hT_in_d, hT_out_d):
        nc = P.nc
        self.P = P
        sb = lambda name, shape, dt: es.enter_context(nc.sbuf_tensor(uniq(name), shape, dt))
        ps = lambda name, shape, dt=F32: es.enter_context(nc.psum_tensor(uniq(name), shape, dt))
        self.ident = sb("ident", [128, 128], F32)
        self.g = sb("lng", [128, D], F32)
        self.b = sb("lnb", [128, D], F32)
        self.hTt = [sb("hTt%d" % i, [128, 16, 128], F32) for i in range(2)]
        self.v = [sb("lnv%d" % i, [128, D], F32) for i in range(2)]
        self.junk = sb("lnjunk", [128, D], BF16)
        self.st = sb("lnst", [128, 8], F32)
        self.oT = [sb("lnoT%d" % i, [128, 16, 128], F32) for i in range(2)]
        self.ptr = [ps("lnptr%d" % i, [128, 512]) for i in range(2)]
        self.hT_in = hT_in_d.rearrange("(kt p) t -> p kt t", p=128)
        self.hT_out = hT_out_d.rearrange("(kt p) t -> p kt t", p=128)
        self.n = 0
        self.np_ = 0
        P.dma(self.ident[:], ident_d[:, :], writes=["ident"])
        P.dma(self.g[:], g_rep_d[:, :], writes=["lng"])
        P.dma(self.b[:], b_rep_d[:, :], writes=["lnb"])

    def load_h(self, t0, s):
        i = self.n % 2
        self.P.dma(self.hTt[i][:, :, :s], self.hT_in[:, :, t0:t0 + s], writes=[("hTt", i)])
        return i

    def run(self, t0, s, mix, mixkeys, hi=None):
        P = self.P
        if hi is None:
            hi = self.load_h(t0, s)
        i = self.n % 2
        self.n += 1
        hTt, v, oT, st = self.hTt[hi], self.v[i], self.oT[i], self.st
        for c in range(4):
            pt = self.ptr[self.np_ % 2]
            pk = ("lnptr", self.np_ % 2)
            self.np_ += 1
            for j in range(4):
                kt = 4 * c + j
                P.emit("pe", lambda e, pt=pt, j=j, kt=kt: e.transpose(out=pt[:s, j * 128:(j + 1) * 128], in_=hTt[:, kt, :s],
                                                                      identity=self.ident[:]),
                       reads=[("hTt", hi), "ident"], writes=[pk])
            P.emit("dve", lambda e, pt=pt, c=c: e.scalar_tensor_tensor(out=v[:s, c * 512:(c + 1) * 512], in0=pt[:s, :], scalar=ALPHA,
                                                                      in1=mix[:s, c * 512:(c + 1) * 512], op0=ALU.mult, op1=ALU.add),
                   reads=[pk] + list(mixkeys), writes=[("lnv", i)])
        P.emit("act", lambda e: e.activation(out=self.junk[:s, :], in_=v[:s, :], func=AF.Copy, accum_out=st[:s, 0:1]),
               reads=[("lnv", i)], writes=["lnjunk", "lnst0"])
        P.emit("act", lambda e: e.activation(out=self.junk[:s, :], in_=v[:s, :], func=AF.Square, accum_out=st[:s, 1:2]),
               reads=[("lnv", i)], writes=["lnjunk", "lnst1"])
        P.emit("dve", lambda e: e.tensor_scalar(out=st[:s, 2:3], in0=st[:s, 0:1], scalar1=1.0 / D, scalar2=None, op0=ALU.mult),
               reads=["lnst0"], writes=["lnst2"])
        P.emit("dve", lambda e: e.tensor_tensor(out=st[:s, 3:4], in0=st[:s, 2:3], in1=st[:s, 2:3], op=ALU.mult),
               reads=["lnst2"], writes=["lnst3"])
        P.emit("dve", lambda e: e.scalar_tensor_tensor(out=st[:s, 4:5], in0=st[:s, 1:2], scalar=1.0 / D, in1=st[:s, 3:4],
                                                       op0=ALU.mult, op1=ALU.subtract), reads=["lnst1", "lnst3"], writes=["lnst4"])
        P.emit("dve", lambda e: e.tensor_scalar(out=st[:s, 4:5], in0=st[:s, 4:5], scalar1=LN_EPS, scalar2=None, op0=ALU.add),
               reads=["lnst4"], writes=["lnst4"])
        P.emit("act", lambda e: e.activation(out=st[:s, 5:6], in_=st[:s, 4:5], func=AF.Sqrt), reads=["lnst4"], writes=["lnst5"])
        P.emit("dve", lambda e: e.reciprocal(out=st[:s, 6:7], in_=st[:s, 5:6]), reads=["lnst5"], writes=["lnst6"])
        P.emit("dve", lambda e: e.tensor_scalar(out=v[:s, :], in0=v[:s, :], scalar1=st[:s, 2:3], scalar2=st[:s, 6:7],
                                                op0=ALU.subtract, op1=ALU.mult), reads=[("lnv", i), "lnst2", "lnst6"], writes=[("lnv", i)])
        P.emit("pool", lambda e: e.tensor_tensor(out=v[:s, :], in0=v[:s, :], in1=self.g[:s, :], op=ALU.mult),
               reads=[("lnv", i), "lng"], writes=[("lnv", i)])
        P.emit("dve", lambda e: e.tensor_tensor(out=v[:s, :], in0=v[:s, :], in1=self.b[:s, :], op=ALU.add),
               reads=[("lnv", i), "lnb"], writes=[("lnv", i)])
        for c in range(4):
            pt = self.ptr[self.np_ % 2]
            pk = ("lnptr", self.np_ % 2)
            self.np_ += 1
            for j in range(4):
                kt = 4 * c + j
                P.emit("pe", lambda e, pt=pt, j=j, kt=kt: e.transpose(out=pt[:, j * 128:j * 128 + s], in_=v[:s, kt * 128:(kt + 1) * 128],
                                                                      identity=self.ident[:s, :s]),
                       reads=[("lnv", i), "ident"], writes=[pk])
            src = pt[:, :].rearrange("p (j t) -> p j t", t=128)[:, :, :s]
            P.emit("act", lambda e, src=src, c=c: e.activation(out=oT[:, 4 * c:4 * c + 4, :s], in_=src, func=AF.Copy),
                   reads=[pk], writes=[("lnoT", i)])
        P.dma(self.hT_out[:, :, t0:t0 + s], oT[:, :, :s], reads=[("lnoT", i)], writes=["hT_out_dram"])


def stage_outproj_ln(P, in_parts, w_out, ident_d, g_rep_d, b_rep_d, hT_in_d, hT_out_d):
    nc = P.nc
    with ExitStack() as es:
        sb = lambda name, shape, dt: es.enter_context(nc.sbuf_tensor(uniq(name), shape, dt))
        ps = lambda name, shape, dt=F32: es.enter_context(nc.psum_tensor(uniq(name), shape, dt))
        ln = LNCtx(P, es, ident_d, g_rep_d, b_rep_d, hT_in_d, hT_out_d)
        wo = sb("wo", [128, 16, D], BF16)
        xin = [sb("xin%d" % i, [128, 16, 128], BF16) for i in range(2)]
        mix = [sb("mix%d" % i, [128, D], F32) for i in range(2)]
        pm = [ps("pm%d" % i, [128, 512]) for i in range(4)]
        for kt in range(16):
            P.dma(wo[:, kt, :], w_out[kt * 128:(kt + 1) * 128, :], writes=["wo"], eng="pool")
        for ti, (t0, s) in enumerate(ttiles()):
            i = ti % 2
            for kt in range(16):
                P.dma(xin[i][:, kt, :s], in_parts[kt][:, t0:t0 + s], writes=[("xin", i)])
            for c in range(4):
                for kt in range(16):
                    P.emit("pe", lambda e, c=c, kt=kt: e.matmul(pm[c][:s, :], lhsT=xin[i][:, kt, :s], rhs=wo[:, kt, c * 512:(c + 1) * 512],
                                                                 start=(kt == 0), stop=(kt == 15)),
                           reads=[("xin", i), "wo"], writes=[("pm", c)])
                P.emit("act", lambda e, c=c: e.activation(out=mix[i][:s, c * 512:(c + 1) * 512], in_=pm[c][:s, :], func=AF.Copy),
                       reads=[("pm", c)], writes=[("mix", i)])
            ln.run(t0, s, mix[i], [("mix", i)])
        P.barrier()


def stage_moe_ln(P, hT_in_d, wr_d, br_d, wguF_d, wdF_d, ident_d, g_rep_d, b_rep_d, lstrict_d, xbuf_d, ybuf_d, hT_out_d, wtag=0):
    nc = P.nc
    tiles = ttiles()
    NT = len(tiles)
    with ExitStack() as es:
        sb = lambda name, shape, dt: es.enter_context(nc.sbuf_tensor(uniq(name), shape, dt))
        ps = lambda name, shape, dt=F32: es.enter_context(nc.psum_tensor(uniq(name), shape, dt))
        oh = sb("oh", [128, 2, NT, 64], F32)
        wts = sb("wts", [128, 2, NT], F32)
        Abf = sb("Abf", [128, NT, 64], BF16)
        dest = sb("dest", [128, 2, NT], I32)
        idxw = sb("idxw", [128, NBLK], I32)
        identb = sb("identb", [128, 128], BF16)
        P.dma(identb[:], ident_d[:, :], writes=["identb"], eng="pool")

        with ExitStack() as e1:
            sb1 = lambda name, shape, dt: e1.enter_context(nc.sbuf_tensor(uniq(name), shape, dt))
            ps1 = lambda name, shape, dt=F32: e1.enter_context(nc.psum_tensor(uniq(name), shape, dt))
            wr = sb1("wr", [128, 16, 72], F32)
            brep = sb1("brep", [128, 72], F32)
            ident = sb1("identf", [128, 128], F32)
            lstr = sb1("lstr", [128, 128], BF16)
            onesb = sb1("onesb", [128, 128], BF16)
            ones64 = sb1("ones64", [128, 64], F32)
            hTt = [sb1("rhTt%d" % i, [128, 16, 128], F32) for i in range(2)]
            htok = sb1("htok", [128, NT, D], BF16)
            lg = sb1("lg", [128, 72], F32)
            em = sb1("em", [128, 64], F32)
            sm = sb1("sm", [128, 16], F32)
            pen = sb1("pen", [128, 8], F32)
            junk = sb1("rjunk", [128, 64], F32)
            cnt = sb1("cnt", [128, 64], F32)
            nb = sb1("nb", [128, 64], F32)
            nbi = sb1("nbi", [128, 64], I32)
            pend = sb1("pend", [128, 64], F32)
            pstart = sb1("pstart", [128, 64], F32)
            base = sb1("base", [128, 64], F32)
            destf = sb1("destf", [128, 2], F32)
            iob = sb1("iob", [128, NBLK], F32)
            cmp = sb1("cmp", [128, NBLK, 64], F32)
            berep = sb1("berep", [128, NBLK], F32)
            iop = sb1("iop", [128, 1], F32)
            plg = ps1("plg", [128, 72])
            pcnt = ps1("pcnt", [128, 64])
            prank = ps1("prank", [128, 64])
            ptr = [ps1("rptr%d" % i, [128, 512]) for i in range(2)]

            P.dma(wr[:], wr_d.rearrange("(kt p) n -> p kt n", p=128), writes=["wr"])
            P.dma(brep[:], br_d[:, :], writes=["brep"])
            P.dma(ident[:], ident_d[:, :], writes=["identf"])
            P.dma(lstr[:], lstrict_d[:, :], writes=["lstr"], eng="pool")
            P.emit("pool", lambda e: e.memset(onesb[:], 1.0), writes=["onesb"])
            P.emit("pool", lambda e: e.memset(ones64[:], 1.0), writes=["ones64"])
            P.emit("pool", lambda e: e.memset(Abf[:], 0.0), writes=["Abf"])
            P.emit("pool", lambda e: e.memset(dest[:], 2 ** 30), writes=["dest"])
            hT_in = hT_in_d.rearrange("(kt p) t -> p kt t", p=128)
            npt = 0
            for ti, (t0, s) in enumerate(tiles):
                i = ti % 2
                h = hTt[i]
                hk = ("rhTt", i)
                P.dma(h[:, :, :s], hT_in[:, :, t0:t0 + s], writes=[hk])
                for kt in range(16):
                    P.emit("pe", lambda e, kt=kt: e.matmul(plg[:s, :], lhsT=h[:, kt, :s], rhs=wr[:, kt, :], start=(kt == 0), stop=(kt == 15)),
                           reads=[hk, "wr"], writes=["plg"])
                P.emit("dve", lambda e: e.tensor_tensor(out=lg[:s, :], in0=plg[:s, :], in1=brep[:s, :], op=ALU.add),
                       reads=["plg", "brep"], writes=["lg"])
                for c in range(4):
                    pt = ptr[npt % 2]
                    pk = ("rptr", npt % 2)
                    npt += 1
                    for j in range(4):
                        kt = 4 * c + j
                        P.emit("pe", lambda e, pt=pt, j=j, kt=kt: e.transpose(out=pt[:s, j * 128:(j + 1) * 128], in_=h[:, kt, :s],
                                                                              identity=ident[:]), reads=[hk, "identf"], writes=[pk])
                    P.emit("act", lambda e, pt=pt, c=c: e.activation(out=htok[:s, ti, c * 512:(c + 1) * 512], in_=pt[:s, :], func=AF.Copy),
                           reads=[pk], writes=["htok"])
                D_ = lambda fn, rd, wr_: P.emit("dve", fn, reads=rd, writes=wr_)
                D_(lambda e: e.tensor_reduce(out=sm[:s, 0:1], in_=lg[:s, 0:8], axis=AX.X, op=ALU.max), ["lg"], ["sm0"])
                D_(lambda e: e.tensor_scalar(out=pen[:s, :], in0=lg[:s, 0:8], scalar1=sm[:s, 0:1], scalar2=None, op0=ALU.is_equal),
                   ["lg", "sm0"], ["pen"])
                D_(lambda e: e.tensor_scalar(out=sm[:s, 1:2], in0=sm[:s, 0:1], scalar1=-1.0, scalar2=None, op0=ALU.mult), ["sm0"], ["sm1"])
                P.emit("act", lambda e: e.activation(out=junk[:s, 0:8], in_=lg[:s, 0:8], func=AF.Exp, bias=sm[:s, 1:2], accum_out=sm[:s, 2:3]),
                       reads=["lg", "sm1"], writes=["rjunk", "sm2"])
                D_(lambda e: e.reciprocal(out=sm[:s, 3:4], in_=sm[:s, 2:3]), ["sm2"], ["sm3"])
                D_(lambda e: e.tensor_scalar(out=pen[:s, :], in0=pen[:s, :], scalar1=BIG, scalar2=-BIG, op0=ALU.mult, op1=ALU.add),
                   ["pen"], ["pen"])
                D_(lambda e: e.tensor_tensor(out=em[:s, :].rearrange("p (g e) -> p g e", g=8),
                                             in0=lg[:s, 8:72].rearrange("p (g e) -> p g e", g=8),
                                             in1=pen[:s, :].unsqueeze(2).to_broadcast([s, 8, 8]), op=ALU.add), ["lg", "pen"], ["em"])
                o1 = oh[:s, 0, ti, :]
                o2 = oh[:s, 1, ti, :]
                D_(lambda e: e.tensor_reduce(out=sm[:s, 4:5], in_=em[:s, :], axis=AX.X, op=ALU.max), ["em"], ["sm4"])
                D_(lambda e: e.tensor_scalar(out=o1, in0=em[:s, :], scalar1=sm[:s, 4:5], scalar2=None, op0=ALU.is_equal), ["em", "sm4"], ["oh"])
                D_(lambda e: e.scalar_tensor_tensor(out=em[:s, :], in0=o1, scalar=-BIG, in1=em[:s, :], op0=ALU.mult, op1=ALU.add),
                   ["oh", "em"], ["em"])
                D_(lambda e: e.tensor_reduce(out=sm[:s, 5:6], in_=em[:s, :], axis=AX.X, op=ALU.max), ["em"], ["sm5"])
                D_(lambda e: e.tensor_scalar(out=o2, in0=em[:s, :], scalar1=sm[:s, 5:6], scalar2=None, op0=ALU.is_equal), ["em", "sm5"], ["oh"])
                D_(lambda e: e.tensor_tensor(out=sm[:s, 6:7], in0=sm[:s, 5:6], in1=sm[:s, 4:5], op=ALU.subtract), ["sm4", "sm5"], ["sm6"])
                P.emit("act", lambda e: e.activation(out=sm[:s, 7:8], in_=sm[:s, 6:7], func=AF.Exp), reads=["sm6"], writes=["sm7"])
                D_(lambda e: e.tensor_scalar(out=sm[:s, 8:9], in0=sm[:s, 7:8], scalar1=1.0, scalar2=None, op0=ALU.add), ["sm7"], ["sm8"])
                D_(lambda e: e.reciprocal(out=sm[:s, 9:10], in_=sm[:s, 8:9]), ["sm8"], ["sm9"])
                D_(lambda e: e.tensor_tensor(out=wts[:s, 0, ti:ti + 1], in0=sm[:s, 9:10], in1=sm[:s, 3:4], op=ALU.mult), ["sm9", "sm3"], ["wts"])
                D_(lambda e: e.tensor_tensor(out=wts[:s, 1, ti:ti + 1], in0=wts[:s, 0, ti:ti + 1], in1=sm[:s, 7:8], op=ALU.mult),
                   ["wts", "sm7"], ["wts"])
                D_(lambda e: e.tensor_tensor(out=Abf[:s, ti, :], in0=o1, in1=o2, op=ALU.add), ["oh"], ["Abf"])
            for ti in range(NT):
                P.emit("pe", lambda e, ti=ti: e.matmul(pcnt[:, :], lhsT=onesb[:, :], rhs=Abf[:, ti, :], start=(ti == 0), stop=(ti == NT - 1)),
                       reads=["onesb", "Abf"], writes=["pcnt"])
            D_ = lambda fn, rd, wr_: P.emit("dve", fn, reads=rd, writes=wr_)
            D_(lambda e: e.tensor_copy(out=cnt[:], in_=pcnt[:]), ["pcnt"], ["cnt"])
            D_(lambda e: e.tensor_scalar(out=nb[:], in0=cnt[:], scalar1=1.0 / 128, scalar2=127.0 / 128 - 0.5 + 1.0 / 256,
                                         op0=ALU.mult, op1=ALU.add), ["cnt"], ["nb"])
            D_(lambda e: e.tensor_copy(out=nbi[:], in_=nb[:]), ["nb"], ["nbi"])
            D_(lambda e: e.tensor_copy(out=nb[:], in_=nbi[:]), ["nbi"], ["nb"])
            D_(lambda e: e.tensor_tensor_scan(out=pend[:], data0=ones64[:], data1=nb[:], initial=0.0, op0=ALU.mult, op1=ALU.add),
               ["ones64", "nb"], ["pend"])
            D_(lambda e: e.tensor_tensor(out=pstart[:], in0=pend[:], in1=nb[:], op=ALU.subtract), ["pend", "nb"], ["pstart"])
            D_(lambda e: e.tensor_scalar(out=pstart[:], in0=pstart[:], scalar1=128.0, scalar2=None, op0=ALU.mult), ["pstart"], ["pstart"])
            for ti, (t0, s) in enumerate(tiles):
                nmm = ti + 1
                for tj in range(ti):
                    P.emit("pe", lambda e, tj=tj: e.matmul(prank[:s, :], lhsT=onesb[:, :s], rhs=Abf[:, tj, :], start=(tj == 0), stop=False),
                           reads=["onesb", "Abf"], writes=["prank"])
                P.emit("pe", lambda e, ti=ti: e.matmul(prank[:s, :], lhsT=lstr[:, :s], rhs=Abf[:, ti, :], start=(ti == 0), stop=True),
                       reads=["lstr", "Abf"], writes=["prank"])
                D_(lambda e: e.tensor_tensor(out=base[:s, :], in0=prank[:s, :], in1=pstart[:s, :], op=ALU.add), ["prank", "pstart"], ["base"])
                for k in range(2):
                    D_(lambda e, k=k: e.tensor_tensor(out=junk[:s, :], in0=base[:s, :], in1=oh[:s, k, ti, :], op=ALU.mult),
                       ["base", "oh"], ["rjunk"])
                    D_(lambda e, k=k: e.tensor_reduce(out=destf[:s, k:k + 1], in_=junk[:s, :], axis=AX.X, op=ALU.add), ["rjunk"], ["destf"])
                D_(lambda e: e.tensor_copy(out=dest[:s, :, ti], in_=destf[:s, :]), ["destf"], ["dest"])
                for k in range(2):
                    P.emit("pool", lambda e, k=k, ti=ti: e.indirect_dma_start(
                        out=xbuf_d[:, :], out_offset=bass.IndirectOffsetOnAxis(ap=dest[:, k, ti:ti + 1], axis=0),
                        in_=htok[:, ti, :], in_offset=None, bounds_check=P.bound_reg(NBLK * 128 - 1), oob_is_err=False),
                        reads=["dest", "htok"], writes=["xbuf"], dma=True)
            P.emit("pool", lambda e: e.iota(iob[:], [[1, NBLK]], base=0, channel_multiplier=0, allow_small_or_imprecise_dtypes=True),
                   writes=["iob"])
            D_(lambda e: e.tensor_tensor(out=cmp[:], in0=pend[:].unsqueeze(1).to_broadcast([128, NBLK, 64]),
                                         in1=iob[:].unsqueeze(2).to_broadcast([128, NBLK, 64]), op=ALU.is_le), ["pend", "iob"], ["cmp"])
            D_(lambda e: e.tensor_reduce(out=berep[:], in_=cmp[:], axis=AX.X, op=ALU.add), ["cmp"], ["berep"])
            P.emit("pool", lambda e: e.iota(iop[:], [[0, 1]], base=0, channel_multiplier=1, allow_small_or_imprecise_dtypes=True),
                   writes=["iop"])
            D_(lambda e: e.tensor_scalar(out=berep[:], in0=berep[:], scalar1=128.0, scalar2=iop[:, 0:1], op0=ALU.mult, op1=ALU.add),
               ["berep", "iop"], ["berep"])
            D_(lambda e: e.tensor_copy(out=idxw[:], in_=berep[:]), ["berep"], ["idxw"])
            P.barrier()

        with ExitStack() as e2:
            sb2 = lambda name, shape, dt: e2.enter_context(nc.sbuf_tensor(uniq(name), shape, dt))
            ps2 = lambda name, shape, dt=F32: e2.enter_context(nc.psum_tensor(uniq(name), shape, dt))
            xblk = [sb2("xblk%d" % i, [128, D], BF16) for i in range(2)]
            xT = [sb2("xT%d" % i, [128, 16, 128], BF16) for i in range(2)]
            wgu = [sb2("wgu%d" % i, [128, 16, 1024], BF16) for i in range(2)]
            wd = [sb2("wd%d" % i, [128, 4, D], BF16) for i in range(2)]
            sg = sb2("sg", [128, 512], F32)
            actT = [sb2("actT%d" % i, [128, 512], BF16) for i in range(2)]
            ybl = [sb2("ybl%d" % i, [128, D], F32) for i in range(2)]
            ptx = [ps2("ptx%d" % i, [128, 1024], BF16) for i in range(2)]
            pg = ps2("pg", [128, 512])
            pu = ps2("pu", [128, 512])
            py = [ps2("py%d" % i, [128, 512]) for i in range(4)]
            P.emit("pool", lambda e: e.memset(wgu[0][:], 0.0), writes=[("wgu", 0)])
            P.emit("pool", lambda e: e.memset(wgu[1][:], 0.0), writes=[("wgu", 1)])
            P.emit("pool", lambda e: e.memset(wd[0][:], 0.0), writes=[("wd", 0)])
            P.emit("pool", lambda e: e.memset(wd[1][:], 0.0), writes=[("wd", 1)])
            for b in range(NBLK):
                i = b % 2
                P.dma(xblk[i][:], xbuf_d[b * 128:(b + 1) * 128, :], reads=["xbuf"], writes=[("xblk", i)])
                P.emit("pool", lambda e, b=b, i=i: e.indirect_dma_start(
                    out=wgu[i][:].rearrange("p a b -> p (a b)"), out_offset=None, in_=wguF_d[:, :],
                    in_offset=bass.IndirectOffsetOnAxis(ap=idxw[:, b:b + 1], axis=0),
                    bounds_check=P.bound_reg(64 * 128 - 1), oob_is_err=False), reads=["idxw", ("wguF", wtag)], writes=[("wgu", i)], dma=True)
                P.emit("pool", lambda e, b=b, i=i: e.indirect_dma_start(
                    out=wd[i][:].rearrange("p a b -> p (a b)"), out_offset=None, in_=wdF_d[:, :],
                    in_offset=bass.IndirectOffsetOnAxis(ap=idxw[:, b:b + 1], axis=0),
                    bounds_check=P.bound_reg(64 * 128 - 1), oob_is_err=False), reads=["idxw", ("wdF", wtag)], writes=[("wd", i)], dma=True)
                for hh in range(2):
                    for j in range(8):
                        kt = 8 * hh + j
                        P.emit("pe", lambda e, hh=hh, j=j, kt=kt: e.transpose(out=ptx[hh][:, j * 128:(j + 1) * 128],
                                                                              in_=xblk[i][:, kt * 128:(kt + 1) * 128], identity=identb[:]),
                               reads=[("xblk", i), "identb"], writes=[("ptx", hh)])
                    P.emit("act", lambda e, hh=hh: e.activation(out=xT[i][:, 8 * hh:8 * hh + 8, :].rearrange("p a b -> p (a b)"),
                                                                in_=ptx[hh][:, :], func=AF.Copy), reads=[("ptx", hh)], writes=[("xT", i)])
                for j in range(4):
                    for kt in range(16):
                        P.emit("pe", lambda e, j=j, kt=kt: e.matmul(pg[:, j * 128:(j + 1) * 128], lhsT=wgu[i][:, kt, j * 128:(j + 1) * 128],
                                                                     rhs=xT[i][:, kt, :], start=(kt == 0), stop=(kt == 15)),
                               reads=[("wgu", i), ("xT", i)], writes=["pg"])
                for j in range(4):
                    for kt in range(16):
                        P.emit("pe", lambda e, j=j, kt=kt: e.matmul(pu[:, j * 128:(j + 1) * 128],
                                                                     lhsT=wgu[i][:, kt, 512 + j * 128:512 + (j + 1) * 128],
                                                                     rhs=xT[i][:, kt, :], start=(kt == 0), stop=(kt == 15)),
                               reads=[("wgu", i), ("xT", i)], writes=["pu"])
                P.emit("act", lambda e: e.activation(out=sg[:], in_=pg[:], func=AF.Silu), reads=["pg"], writes=["sg"])
                P.emit("dve", lambda e: e.tensor_tensor(out=actT[i][:], in0=sg[:], in1=pu[:], op=ALU.mult), reads=["sg", "pu"], writes=[("actT", i)])
                for c in range(4):
                    for kt in range(4):
                        P.emit("pe", lambda e, c=c, kt=kt: e.matmul(py[c][:, :], lhsT=actT[i][:, kt * 128:(kt + 1) * 128],
                                                                     rhs=wd[i][:, kt, c * 512:(c + 1) * 512], start=(kt == 0), stop=(kt == 3)),
                               reads=[("actT", i), ("wd", i)], writes=[("py", c)])
                    eng = "act" if c % 2 == 0 else "dve"
                    if eng == "act":
                        P.emit("act", lambda e, c=c: e.activation(out=ybl[i][:, c * 512:(c + 1) * 512], in_=py[c][:, :], func=AF.Copy),
                               reads=[("py", c)], writes=[("ybl", i)])
                    else:
                        P.emit("dve", lambda e, c=c: e.tensor_copy(out=ybl[i][:, c * 512:(c + 1) * 512], in_=py[c][:, :]),
                               reads=[("py", c)], writes=[("ybl", i)])
                P.dma(ybuf_d[b * 128:(b + 1) * 128, :], ybl[i][:], reads=[("ybl", i)], writes=["ybuf"])
            P.barrier()

        with ExitStack() as e3:
            sb3 = lambda name, shape, dt: e3.enter_context(nc.sbuf_tensor(uniq(name), shape, dt))
            ln = LNCtx(P, e3, ident_d, g_rep_d, b_rep_d, hT_in_d, hT_out_d)
            y1 = [sb3("y1%d" % i, [128, D], F32) for i in range(2)]
            y2 = [sb3("y2%d" % i, [128, D], F32) for i in range(2)]
            for ti, (t0, s) in enumerate(tiles):
                i = ti % 2
                for k, yb in ((0, y1[i]), (1, y2[i])):
                    P.emit("pool", lambda e, k=k, yb=yb, ti=ti: e.indirect_dma_start(
                        out=yb[:, :], out_offset=None, in_=ybuf_d[:, :],
                        in_offset=bass.IndirectOffsetOnAxis(ap=dest[:, k, ti:ti + 1], axis=0),
                        bounds_check=P.bound_reg(NBLK * 128 - 1), oob_is_err=False), reads=["dest", "ybuf"], writes=[("y%d" % k, i)], dma=True)
                P.emit("dve", lambda e, ti=ti: e.tensor_scalar(out=y1[i][:s, :], in0=y1[i][:s, :], scalar1=wts[:s, 0, ti:ti + 1], scalar2=None,
                                                               op0=ALU.mult), reads=[("y0", i), "wts"], writes=[("y0", i)])
                P.emit("pool", lambda e, ti=ti: e.tensor_scalar(out=y2[i][:s, :], in0=y2[i][:s, :], scalar1=wts[:s, 1, ti:ti + 1], scalar2=None,
                                                                op0=ALU.mult), reads=[("y1", i), "wts"], writes=[("y1", i)])
                P.emit("dve", lambda e: e.tensor_tensor(out=y1[i][:s, :], in0=y1[i][:s, :], in1=y2[i][:s, :], op=ALU.add),
                       reads=[("y0", i), ("y1", i)], writes=[("y0", i)])
                ln.run(t0, s, y1[i], [("y0", i)])
            P.barrier()


T = 2050
D = 2048
CH = 82
NCH = T // CH
RMS_EPS = 1e-6


def stage_inproj_c(P, hT_d, w_in_c, qT_d, fT_d, gT_d, v_d):
    nc = P.nc
    with ExitStack() as es:
        sb = lambda name, shape, dt: es.enter_context(nc.sbuf_tensor(uniq(name), shape, dt))
        ps = lambda name, shape, dt=F32: es.enter_context(nc.psum_tensor(uniq(name), shape, dt))
        hb = sb("hb", [128, 16, T], BF16)
        wj = sb("wj", [128, 16, D], BF16)
        of = [sb("of%d" % i, [128, 512], F32) for i in range(2)]
        ob = [sb("ob%d" % i, [128, 512], BF16) for i in range(2)]
        vb = [sb("vb%d" % i, [128, D], BF16) for i in range(2)]
        pp = [ps("pp%d" % i, [128, 512]) for i in range(4)]
        hv = hT_d.rearrange("(kt p) t -> p kt t", p=128)
        for kt in range(16):
            P.dma(hb[:, kt, :], hv[:, kt, :], writes=["hb"], eng="pool")
        blocks = [(b * 410, 410) for b in range(5)]
        ttl = [(i * 128, 128) for i in range(16)] + [(2048, 2)]
        n = 0
        for j in range(5):
            for kt in range(16):
                P.dma(wj[:, kt, :], w_in_c[kt * 128:(kt + 1) * 128, j * D:(j + 1) * D], writes=["wj"], eng="pool")
            if j == 1:
                for ti, (t0, s) in enumerate(ttl):
                    i = ti % 2
                    for c in range(4):
                        for kt in range(16):
                            P.emit("pe", lambda e, c=c, kt=kt: e.matmul(pp[c][:s, :], lhsT=hb[:, kt, t0:t0 + s], rhs=wj[:, kt, c * 512:(c + 1) * 512],
                                                                         start=(kt == 0), stop=(kt == 15)), reads=["hb", "wj"], writes=[("pp", c)])
                        P.emit("act", lambda e, c=c: e.activation(out=vb[i][:s, c * 512:(c + 1) * 512], in_=pp[c][:s, :], func=AF.Copy),
                               reads=[("pp", c)], writes=[("vb", i)])
                    P.dma(v_d[t0:t0 + s, :], vb[i][:s, :], reads=[("vb", i)], writes=["v_dram"])
                continue
            for (c0, nb) in blocks:
                for m in range(16):
                    pi = n % 4
                    i = n % 2
                    n += 1
                    for kt in range(16):
                        P.emit("pe", lambda e, m=m, kt=kt, pi=pi: e.matmul(pp[pi][:, :nb], lhsT=wj[:, kt, m * 128:(m + 1) * 128], rhs=hb[:, kt, c0:c0 + nb],
                                                                            start=(kt == 0), stop=(kt == 15)), reads=["hb", "wj"], writes=[("pp", pi)])
                    if j in (2, 3):
                        P.emit("act", lambda e, pi=pi: e.activation(out=of[i][:, :nb], in_=pp[pi][:, :nb], func=AF.Copy),
                               reads=[("pp", pi)], writes=[("of", i)])
                        P.dma(fT_d[j - 2, m * 128:(m + 1) * 128, c0:c0 + nb], of[i][:, :nb], reads=[("of", i)], writes=["f_dram"])
                    else:
                        dst = qT_d if j == 0 else gT_d
                        P.emit("act", lambda e, pi=pi: e.activation(out=ob[i][:, :nb], in_=pp[pi][:, :nb], func=AF.Copy),
                               reads=[("pp", pi)], writes=[("ob", i)])
                        P.dma(dst[m * 128:(m + 1) * 128, c0:c0 + nb], ob[i][:, :nb], reads=[("ob", i)], writes=["qg_dram"])
        P.barrier()


def stage_hgrn_scan(P, qT_d, fT_d, v_d, lbT_d, ident_d, maskF_d, maskB_d, S_in_d, S_out_d, D_out_d, oF_d, oB_d, full):
    nc = P.nc
    with ExitStack() as es:
        sb = lambda name, shape, dt: es.enter_context(nc.sbuf_tensor(uniq(name), shape, dt))
        ps = lambda name, shape, dt=F32: es.enter_context(nc.psum_tensor(uniq(name), shape, dt))
        lb = sb("lb", [128, 16], F32)
        oml = sb("oml", [128, 16], F32)
        identb = sb("identb", [128, 128], BF16)
        mk = [sb("mk%d" % d, [128, 128], F32) for d in range(2)]
        rst = [sb("rst%d" % d, [128, 16, CH], F32) for d in range(2)]
        S = sb("S", [128, 2, 16, 128], F32)
        Sb = sb("Sb", [128, 2, 16, 128], BF16)
        Dt = sb("Dt", [128, 2, 16], F32)
        fz = [sb("fz%d" % d, [128, 16, CH], F32) for d in range(2)]
        ff = [sb("ff%d" % d, [128, 16, CH], F32) for d in range(2)]
        gg = [sb("gg%d" % d, [128, 16, CH], F32) for d in range(2)]
        bb = [sb("bb%d" % d, [128, 16, CH], F32) for d in range(2)]
        eb = [sb("eb%d" % d, [128, 16, CH], F32) for d in range(2)]
        enb = [sb("enb%d" % d, [128, 16, CH], F32) for d in range(2)]
        ka = [sb("ka%d" % d, [128, 16, CH], BF16) for d in range(2)]
        kh = [sb("kh%d" % d, [128, 16, CH], BF16) for d in range(2)]
        eend = [sb("eend%d" % d, [128, 16], F32) for d in range(2)]
        khT = [sb("khT%d" % i, [128, 128], BF16) for i in range(2)]
        vt = [sb("vt%d" % d, [128, D], BF16) for d in range(2)]
        if full:
            qb = [sb("qb%d" % d, [128, 16, CH], BF16) for d in range(2)]
            qa = [sb("qa%d" % d, [128, 16, CH], BF16) for d in range(2)]
            sc = [sb("sc%d" % i, [128, CH], BF16) for i in range(2)]
            ost = [sb("ost%d" % d, [128, 16, CH], F32) for d in range(2)]
            psc = [ps("psc%d" % i, [128, 512]) for i in range(2)]
            pout = [ps("pout%d" % i, [128, 512]) for i in range(2)]
        ptr = [ps("ptr%d" % i, [128, 1024], BF16) for i in range(2)]
        pst = [ps("pst%d" % i, [128, 512]) for i in range(2)]

        P.dma(lb[:], lbT_d[:, :], writes=["lb"])
        P.dma(identb[:], ident_d[:, :], writes=["identb"], eng="pool")
        P.dma(mk[0][:], maskF_d[:, :], writes=["mk"])
        P.dma(mk[1][:], maskB_d[:, :], writes=["mk"])
        P.emit("dve", lambda e: e.tensor_scalar(out=oml[:], in0=lb[:], scalar1=-1.0, scalar2=1.0, op0=ALU.mult, op1=ALU.add),
               reads=["lb"], writes=["oml"])
        for d in range(2):
            P.emit("pool", lambda e, d=d: e.memset(rst[d][:], 1.0), writes=[("rst", d)])
            col = 0 if d == 0 else CH - 1
            P.emit("pool", lambda e, d=d, col=col: e.memset(rst[d][:, :, col:col + 1], 0.0), writes=[("rst", d)])
            P.emit("pool", lambda e, d=d: e.memset(Dt[:, d, :], 1.0), writes=["Dt"])
        if S_in_d is not None:
            for d in range(2):
                P.dma(S[:, d].rearrange("p h v -> p (h v)"), S_in_d[d], writes=[("S", d, h) for h in range(16)])
        else:
            P.emit("pool", lambda e: e.memset(S[:], 0.0), writes=[("S", d, h) for d in range(2) for h in range(16)])
        P.emit("act", lambda e: e.activation(out=Sb[:], in_=S[:], func=AF.Copy), reads=[("S", d, h) for d in range(2) for h in range(16)],
               writes=[("Sb", d, h) for d in range(2) for h in range(16)])

        fv = [fT_d[d].rearrange("(h p) t -> p h t", p=128) for d in range(2)]
        qv = qT_d.rearrange("(h p) t -> p h t", p=128)
        nt = 0
        for ci in range(NCH):
            for d in range(2):
                c = ci if d == 0 else NCH - 1 - ci
                t0 = c * CH
                dk = lambda s: (s, d)
                P.dma(fz[d][:], fv[d][:, :, t0:t0 + CH], writes=[dk("fz")])
                P.dma(vt[d][:CH, :], v_d[t0:t0 + CH, :], writes=[dk("vt")])
                P.emit("act", lambda e, d=d: e.activation(out=ff[d][:], in_=fz[d][:], func=AF.Sigmoid), reads=[dk("fz")], writes=[dk("ff")])
                P.emit("dve", lambda e, d=d: e.tensor_tensor(out=ff[d][:], in0=ff[d][:], in1=oml[:].unsqueeze(2).to_broadcast([128, 16, CH]),
                                                             op=ALU.mult), reads=[dk("ff"), "oml"], writes=[dk("ff")])
                P.emit("dve", lambda e, d=d: e.tensor_tensor(out=ff[d][:], in0=ff[d][:], in1=lb[:].unsqueeze(2).to_broadcast([128, 16, CH]),
                                                             op=ALU.add), reads=[dk("ff"), "lb"], writes=[dk("ff")])
                P.emit("act", lambda e, d=d: e.activation(out=gg[d][:], in_=ff[d][:], func=AF.Ln), reads=[dk("ff")], writes=[dk("gg")])
                flat = lambda tl: tl[:].rearrange("p h t -> p (h t)")
                if d == 0:
                    P.emit("dve", lambda e, d=d: e.tensor_tensor_scan(out=flat(bb[d]), data0=flat(rst[d]), data1=flat(gg[d]), initial=0.0,
                                                                      op0=ALU.mult, op1=ALU.add), reads=[dk("gg"), dk("rst")], writes=[dk("bb")])
                else:
                    P.emit("dve", lambda e, d=d: e.tensor_tensor_scan(out=flat(bb[d])[:, ::-1], data0=flat(rst[d])[:, ::-1], data1=flat(gg[d])[:, ::-1],
                                                                      initial=0.0, op0=ALU.mult, op1=ALU.add),
                           reads=[dk("gg"), dk("rst")], writes=[dk("bb")])
                P.emit("act", lambda e, d=d: e.activation(out=eb[d][:], in_=bb[d][:], func=AF.Exp), reads=[dk("bb")], writes=[dk("eb")])
                P.emit("act", lambda e, d=d: e.activation(out=enb[d][:], in_=bb[d][:], func=AF.Exp, scale=-1.0), reads=[dk("bb")], writes=[dk("enb")])
                P.emit("pool", lambda e, d=d: e.tensor_scalar(out=ff[d][:], in0=ff[d][:], scalar1=-1.0, scalar2=1.0, op0=ALU.mult, op1=ALU.add),
                       reads=[dk("ff")], writes=[dk("ff")])
                P.emit("dve", lambda e, d=d: e.tensor_tensor(out=ka[d][:], in0=ff[d][:], in1=enb[d][:], op=ALU.mult),
                       reads=[dk("ff"), dk("enb")], writes=[dk("ka")])
                last = CH - 1 if d == 0 else 0
                P.emit("dve", lambda e, d=d, last=last: e.tensor_copy(out=eend[d][:], in_=eb[d][:, :, last]), reads=[dk("eb")], writes=[dk("eend")])
                P.emit("dve", lambda e, d=d: e.tensor_tensor(out=kh[d][:], in0=ka[d][:], in1=eend[d][:].unsqueeze(2).to_broadcast([128, 16, CH]),
                                                             op=ALU.mult), reads=[dk("ka"), dk("eend")], writes=[dk("kh")])
                P.emit("dve", lambda e, d=d: e.tensor_tensor(out=Dt[:, d, :], in0=Dt[:, d, :], in1=eend[d][:], op=ALU.mult),
                       reads=["Dt", dk("eend")], writes=["Dt"])
                if full:
                    P.dma(qb[d][:], qv[:, :, t0:t0 + CH], writes=[dk("qb")])
                    P.emit("pool", lambda e, d=d: e.tensor_tensor(out=qa[d][:], in0=qb[d][:], in1=eb[d][:], op=ALU.mult),
                           reads=[dk("qb"), dk("eb")], writes=[dk("qa")])
                for h in range(16):
                    i = nt % 2
                    nt += 1
                    if full:
                        P.emit("pe", lambda e, d=d, h=h, i=i: e.matmul(psc[i][:CH, :CH], lhsT=ka[d][:, h, :], rhs=qa[d][:, h, :], start=True, stop=True),
                               reads=[dk("ka"), dk("qa")], writes=[("psc", i)])
                        P.emit("dve", lambda e, d=d, i=i: e.tensor_tensor(out=sc[i][:CH, :], in0=psc[i][:CH, :CH], in1=mk[d][:CH, :CH], op=ALU.mult),
                               reads=[("psc", i), "mk"], writes=[("sc", i)])
                        P.emit("pe", lambda e, d=d, h=h, i=i: e.matmul(pout[i][:, :CH], lhsT=vt[d][:CH, h * 128:(h + 1) * 128], rhs=sc[i][:CH, :],
                                                                        start=True, stop=False), reads=[dk("vt"), ("sc", i)], writes=[("pout", i)])
                        P.emit("pe", lambda e, d=d, h=h, i=i: e.matmul(pout[i][:, :CH], lhsT=Sb[:, d, h, :], rhs=qa[d][:, h, :], start=False, stop=True),
                               reads=[("Sb", d, h), dk("qa")], writes=[("pout", i)])
                        P.emit("act", lambda e, d=d, h=h, i=i: e.activation(out=ost[d][:, h, :], in_=pout[i][:, :CH], func=AF.Copy),
                               reads=[("pout", i)], writes=[dk("ost")])
                    P.emit("pe", lambda e, d=d, h=h, i=i: e.transpose(out=ptr[i][:CH, :128], in_=kh[d][:, h, :], identity=identb[:]),
                           reads=[dk("kh"), "identb"], writes=[("ptr", i)])
                    P.emit("act", lambda e, i=i: e.activation(out=khT[i][:CH, :], in_=ptr[i][:CH, :128], func=AF.Copy),
                           reads=[("ptr", i)], writes=[("khT", i)])
                    P.emit("pe", lambda e, d=d, h=h, i=i: e.matmul(pst[i][:, :128], lhsT=khT[i][:CH, :], rhs=vt[d][:CH, h * 128:(h + 1) * 128],
                                                                    start=True, stop=True), reads=[("khT", i), dk("vt")], writes=[("pst", i)])
                    P.emit("dve", lambda e, d=d, h=h, i=i: e.scalar_tensor_tensor(out=S[:, d, h, :], in0=S[:, d, h, :], scalar=eend[d][:, h:h + 1],
                                                                                  in1=pst[i][:, :128], op0=ALU.mult, op1=ALU.add),
                           reads=[("S", d, h), dk("eend"), ("pst", i)], writes=[("S", d, h)])
                    P.emit("pool", lambda e, d=d, h=h: e.tensor_copy(out=Sb[:, d, h, :], in_=S[:, d, h, :]), reads=[("S", d, h)], writes=[("Sb", d, h)])
                if full:
                    od = oF_d if d == 0 else oB_d
                    P.dma(od.rearrange("(h p) t -> p h t", p=128)[:, :, t0:t0 + CH], ost[d][:], reads=[dk("ost")], writes=["o_dram"])
        for d in range(2):
            P.dma(S_out_d[d], S[:, d].rearrange("p h v -> p (h v)"), reads=[("S", d, h) for h in range(16)], writes=["S_out"])
            P.dma(D_out_d[d], Dt[:, d, :], reads=["Dt"], writes=["D_out"])
        P.barrier()


def stage_hgrn_post(P, oF_d, oB_d, gT_d, ngT_d, oT_d):
    nc = P.nc
    with ExitStack() as es:
        sb = lambda name, shape, dt: es.enter_context(nc.sbuf_tensor(uniq(name), shape, dt))
        ps = lambda name, shape, dt=F32: es.enter_context(nc.psum_tensor(uniq(name), shape, dt))
        ng = sb("ng", [128, 16], F32)
        ones = sb("ones", [128, 128], F32)
        a = [sb("pa%d" % i, [128, 410], F32) for i in range(2)]
        b = [sb("pb%d" % i, [128, 410], F32) for i in range(2)]
        gt = [sb("pg%d" % i, [128, 410], BF16) for i in range(2)]
        sq = [sb("psq%d" % i, [128, 410], F32) for i in range(2)]
        rs = [sb("prs%d" % i, [128, 410], F32) for i in range(2)]
        ob = [sb("pob%d" % i, [128, 410], BF16) for i in range(2)]
        pm = [ps("ppm%d" % i, [128, 410]) for i in range(2)]
        P.dma(ng[:], ngT_d[:, :], writes=["ng"])
        P.emit("pool", lambda e: e.memset(ones[:], 1.0 / 128), writes=["ones"])
        n = 0
        for h in range(16):
            for blk in range(5):
                c0 = blk * 410
                i = n % 2
                n += 1
                rows = slice(h * 128, (h + 1) * 128)
                P.dma(a[i][:], oF_d[rows, c0:c0 + 410], writes=[("a", i)])
                P.dma(b[i][:], oB_d[rows, c0:c0 + 410], writes=[("b", i)])
                P.dma(gt[i][:], gT_d[rows, c0:c0 + 410], writes=[("gt", i)])
                P.emit("dve", lambda e, i=i: e.tensor_tensor(out=a[i][:], in0=a[i][:], in1=b[i][:], op=ALU.add), reads=[("a", i), ("b", i)], writes=[("a", i)])
                P.emit("act", lambda e, i=i: e.activation(out=sq[i][:], in_=a[i][:], func=AF.Square), reads=[("a", i)], writes=[("sq", i)])
                P.emit("pe", lambda e, i=i: e.matmul(pm[i][:, :], lhsT=ones[:], rhs=sq[i][:], start=True, stop=True), reads=["ones", ("sq", i)], writes=[("pm", i)])
                P.emit("dve", lambda e, i=i: e.tensor_scalar(out=rs[i][:], in0=pm[i][:], scalar1=RMS_EPS, scalar2=None, op0=ALU.add),
                       reads=[("pm", i)], writes=[("rs", i)])
                P.emit("act", lambda e, i=i: e.activation(out=rs[i][:], in_=rs[i][:], func=AF.Sqrt), reads=[("rs", i)], writes=[("rs", i)])
                P.emit("dve", lambda e, i=i: e.reciprocal(out=rs[i][:], in_=rs[i][:]), reads=[("rs", i)], writes=[("rs", i)])
                P.emit("act", lambda e, i=i: e.activation(out=b[i][:], in_=gt[i][:], func=AF.Sigmoid), reads=[("gt", i), ("b", i)], writes=[("b", i)])
                P.emit("dve", lambda e, i=i, h=h: e.scalar_tensor_tensor(out=a[i][:], in0=a[i][:], scalar=ng[:, h:h + 1], in1=rs[i][:],
                                                                        op0=ALU.mult, op1=ALU.mult), reads=[("a", i), "ng", ("rs", i)], writes=[("a", i)])
                P.emit("dve", lambda e, i=i: e.tensor_tensor(out=ob[i][:], in0=a[i][:], in1=b[i][:], op=ALU.mult), reads=[("a", i), ("b", i)], writes=[("ob", i)])
                P.dma(oT_d[rows, c0:c0 + 410], ob[i][:], reads=[("ob", i)], writes=["oT_dram"])
        P.barrier()


def stage_hgrn_carry(P, S_all_d, D_all_d, fm_d, bm_d, S_in_d):
    nc = P.nc
    with ExitStack() as es:
        sb = lambda name, shape, dt: es.enter_context(nc.sbuf_tensor(uniq(name), shape, dt))
        S = sb("cS", [128, 16, 128], F32)
        E = [sb("cE%d" % i, [128, 16, 128], F32) for i in range(2)]
        Dc = [sb("cD%d" % i, [128, 16], F32) for i in range(2)]
        mk = sb("cmk", [128, 2, 8], F32)
        P.dma(mk[:, 0, :], fm_d[:, :], writes=["cmk"])
        P.dma(mk[:, 1, :], bm_d[:, :], writes=["cmk"])
        n = 0
        for d in range(2):
            P.emit("pool", lambda e: e.memset(S[:], 0.0), writes=["cS"])
            order = range(8) if d == 0 else range(7, -1, -1)
            for c in order:
                i = n % 2
                n += 1
                P.dma(E[i][:].rearrange("p h v -> p (h v)"), S_all_d[c, d], writes=[("cE", i)])
                P.dma(Dc[i][:], D_all_d[c, d], writes=[("cD", i)])
                m = mk[:, d, c:c + 1]
                P.emit("dve", lambda e, i=i, m=m: e.tensor_scalar(out=Dc[i][:], in0=Dc[i][:], scalar1=-1.0, scalar2=m, op0=ALU.add, op1=ALU.mult),
                       reads=[("cD", i), "cmk"], writes=[("cD", i)])
                P.emit("dve", lambda e, i=i: e.tensor_scalar(out=Dc[i][:], in0=Dc[i][:], scalar1=1.0, scalar2=None, op0=ALU.add),
                       reads=[("cD", i)], writes=[("cD", i)])
                P.emit("pool", lambda e, i=i, m=m: e.tensor_scalar(out=E[i][:], in0=E[i][:], scalar1=m, scalar2=None, op0=ALU.mult),
                       reads=[("cE", i), "cmk"], writes=[("cE", i)])
                for h in range(16):
                    P.emit("dve", lambda e, i=i, h=h: e.scalar_tensor_tensor(out=S[:, h, :], in0=S[:, h, :], scalar=Dc[i][:, h:h + 1], in1=E[i][:, h, :],
                                                                            op0=ALU.mult, op1=ALU.add), reads=["cS", ("cD", i), ("cE", i)], writes=["cS"])
            P.dma(S_in_d[d], S[:].rearrange("p h v -> p (h v)"), reads=["cS"], writes=["S_in_dram"])
        P.barrier()


T = 2050
TH = 2432
L = 16400
NC = 8


def rope_tables(pos):
    half = 64
    inv = (10000.0 ** (-np.arange(half, dtype=np.float32) * 2.0 / 128)).astype(np.float32)
    ang = pos.astype(np.float32)[None, :] * inv[:, None]
    c = np.cos(ang).astype(np.float32)
    s = np.sin(ang).astype(np.float32)
    return np.concatenate([c, c], 0), np.concatenate([-s, s], 0)


def prep_attn_inputs(x, meta_tokens, c):
    h0 = np.concatenate([meta_tokens, x[0]], axis=0)
    lo = T * c - 128
    idx = lo + np.arange(TH)
    ok = (idx >= 0) & (idx < L)
    hT0 = np.zeros((2048, TH), np.float32)
    hT0[:, ok] = h0[idx[ok]].T
    ropeC, ropeS = rope_tables(np.clip(idx, 0, L - 1))
    ropeCm, ropeSm = rope_tables(np.arange(16))
    g = lo + (np.arange(19)[None, :] * 128 + np.arange(128)[:, None])
    kvalid = ((g >= 16) & (g < L)).astype(np.float32)
    kk = np.arange(128)[:, None]
    qq = np.arange(128)[None, :]
    maskL = (kk >= qq).astype(np.float32)
    maskR = (kk <= qq).astype(np.float32)
    perm = np.zeros((128, 128), np.float32)
    for m in range(128):
        perm[(m + 64) % 128, m] = 1.0
    return dict(hT0=hT0, metaT=np.ascontiguousarray(meta_tokens.T), ropeC=ropeC, ropeS=ropeS,
                ropeCm=ropeCm, ropeSm=ropeSm, kvalid=kvalid, maskL=maskL, maskR=maskR, perm=perm)


def prep_s5_inputs(d, j=0):
    lam_re, lam_im, ls = d["s5_lam_re"][j], d["s5_lam_im"][j], d["s5_log_step"][j]
    lamT = np.zeros((2, 3, 128, 32), np.float32)
    for g2 in range(2):
        lamT[:, 0, g2 * 64:(g2 + 1) * 64, :] = lam_re[:, g2::2, :].transpose(0, 2, 1)
        lamT[:, 1, g2 * 64:(g2 + 1) * 64, :] = lam_im[:, g2::2, :].transpose(0, 2, 1)
        lamT[:, 2, g2 * 64:(g2 + 1) * 64, :] = ls[:, g2::2][:, None, :]
    row = lamT.transpose(0, 1, 3, 2).reshape(2, 3, 1, 4096)
    lamR = np.ascontiguousarray(np.broadcast_to(row, (2, 3, 128, 4096)))
    Bl = np.zeros((2, 2, 128, 32, 128), np.float32)
    Cl = np.zeros((2, 2, 128, 32, 128), np.float32)
    B = [d["s5_b_re"][j], d["s5_b_im"][j]]
    C = [d["s5_c_re"][j], d["s5_c_im"][j]]
    for q in range(32):
        for g2 in range(2):
            g = 2 * q + g2
            r0 = 32 * (q % 4) + 16 * g2
            for c in range(2):
                Bl[:, c, r0:r0 + 16, q, 64 * g2:64 * g2 + 64] = B[c][:, g].transpose(0, 2, 1)
                Cl[:, c, 64 * g2:64 * g2 + 64, q, r0:r0 + 16] = C[c][:, g].transpose(0, 2, 1)
    dsk = np.ascontiguousarray(d["s5_d"][j].reshape(8, 128).T)
    bglu = np.ascontiguousarray(d["s5_b_glu"][j].reshape(8, 128).T)
    return dict(lamT=lamT, lamR=lamR, Bl=Bl, Cl=Cl, dsk=dsk, wglu=d["s5_w_glu"][j], bglu=bglu)


def s5_carry(ends, c):
    out = np.zeros((2, 7, 128, 64), np.float32)
    for k in range(7):
        src = c - 7 + k
        if src >= 0:
            out[0, k] = ends[src][0]
        src = c + 7 - k
        if src < NC:
            out[1, k] = ends[src][1]
    return out


def prep_common_consts():
    ident = np.eye(128, dtype=np.float32)
    t = np.arange(128)
    lstrict = (t[:, None] < t[None, :]).astype(np.float32)
    return dict(ident=ident, lstrict=lstrict)


def rep128(v):
    return np.ascontiguousarray(np.broadcast_to(np.asarray(v, np.float32).reshape(1, -1), (128, v.size)))


def prep_moe_inputs(d, layer, pfx):
    wr = np.ascontiguousarray(np.concatenate([d["moe_w_group"][layer], d["moe_w_expert"][layer]], axis=1))
    br = rep128(np.concatenate([d["moe_b_group"][layer], d["moe_b_expert"][layer]]))
    return {pfx + "wr": wr, pfx + "br": br,
            pfx + "wgu": d["moe_w_gate_up"][layer].reshape(64 * 2048, 1024),
            pfx + "wd": d["moe_w_down"][layer].reshape(64 * 512, 2048),
            pfx + "g2": rep128(d["ln_ffn_g"][layer]), pfx + "b2": rep128(d["ln_ffn_b"][layer]),
            pfx + "g1": rep128(d["ln_mix_g"][layer]), pfx + "b1": rep128(d["ln_mix_b"][layer])}


L_TOT = 16400


def stage_moe_wprep(P, wgus_d, wds_d, wgu_sh, wd_sh, wguF, wdF, tag):
    nc = P.nc
    with ExitStack() as es:
        sb = lambda name, shape, dt: es.enter_context(nc.sbuf_tensor(uniq(name), shape, dt))
        stf = [sb("stf%d" % i, [128, 8, 1024], F32) for i in range(2)]
        tb = [sb("tb%d" % i, [128, 8, 1024], BF16) for i in range(3)]
        n = 0
        for e in range(8):
            pieces = []
            for h in range(2):
                pieces.append((wgus_d[e * 2048 + h * 1024:e * 2048 + (h + 1) * 1024, :].rearrange("(kt p) c -> p kt c", p=128),
                               wgu_sh[e * 128:(e + 1) * 128, h * 8192:(h + 1) * 8192].rearrange("p (kt c) -> p kt c", kt=8), 8, 1024, "wgu_sh"))
            pieces.append((wds_d[e * 512:(e + 1) * 512, :].rearrange("(kt p) c -> p kt c", p=128),
                           wd_sh[e * 128:(e + 1) * 128, :].rearrange("p (kt c) -> p kt c", kt=4), 4, 2048, "wd_sh"))
            for (src, dst, a_, b_, dk) in pieces:
                i = n % 2
                j = n % 3
                sv = stf[i][:].rearrange("p a b -> p (a b)").rearrange("p (a b) -> p a b", a=a_)
                tv = tb[j][:].rearrange("p a b -> p (a b)").rearrange("p (a b) -> p a b", a=a_)
                P.dma(sv, src, writes=[("stf", i)])
                eng = ("act", "dve", "pool")[j]
                if eng == "act":
                    P.emit("act", lambda e_, sv=sv, tv=tv: e_.activation(out=tv, in_=sv, func=AF.Copy), reads=[("stf", i)], writes=[("tb", j)])
                else:
                    P.emit(eng, lambda e_, sv=sv, tv=tv: e_.tensor_copy(out=tv, in_=sv), reads=[("stf", i)], writes=[("tb", j)])
                P.dma(dst, tv, reads=[("tb", j)], writes=[dk], eng="act" if n % 2 else "sp")
                n += 1
        P.collective("AllGather", [wgu_sh], [wguF], reads=["wgu_sh"], writes=[("wguF", tag)], bg=True)
        P.collective("AllGather", [wd_sh], [wdF], reads=["wd_sh"], writes=[("wdF", tag)], bg=True)
        P.barrier()


def stage_lb(P, lgT_d, lbT_d):
    nc = P.nc
    with ExitStack() as es:
        lg = es.enter_context(nc.sbuf_tensor(uniq("lg"), [128, 2, 16], F32))
        lb = es.enter_context(nc.sbuf_tensor(uniq("lbt"), [128, 16], F32))
        P.dma(lg[:], lgT_d[:, :, :], writes=["lg"])
        P.emit("dve", lambda e: e.tensor_tensor(out=lb[:], in0=lg[:, 1, :], in1=lg[:, 0, :], op=ALU.subtract), reads=["lg"], writes=["lbt"])
        P.emit("act", lambda e: e.activation(out=lb[:], in_=lb[:], func=AF.Sigmoid), reads=["lbt"], writes=["lbt"])
        P.dma(lbT_d[:, :], lb[:], reads=["lbt"], writes=["lb_dram"])
        P.barrier()


INPUT_SHAPES = dict(
    hT0=[2048, TH], metaT=[2048, 16], w_in=[2048, 2560], ropeC=[128, TH], ropeS=[128, TH], ropeCm=[128, 16], ropeSm=[128, 16],
    perm=[128, 128], kvalid=[128, 19], maskL=[128, 128], maskR=[128, 128], sinks=[128, 8],
    lamT=[2, 3, 128, 32], lamR=[2, 3, 128, 4096], Bl=[2, 2, 128, 32, 128], Cl=[2, 2, 128, 32, 128],
    dsk=[128, 8], wglu=[1024, 1024], bglu=[128, 8], fm=[128, 8], bm=[128, 8],
    w_out0=[2048, 2048], w_out1=[2048, 2048], ident=[128, 128], lstrict=[128, 128],
    w_in_c=[2048, 10240], lgT=[128, 2, 16], ngT=[128, 16],
)
for _l in range(2):
    INPUT_SHAPES.update({"m%dwr" % _l: [2048, 72], "m%dbr" % _l: [128, 72], "m%dwgus" % _l: [8 * 2048, 1024], "m%dwds" % _l: [8 * 512, 2048],
                         "m%dg1" % _l: [128, 2048], "m%db1" % _l: [128, 2048], "m%dg2" % _l: [128, 2048], "m%db2" % _l: [128, 2048]})


def build_program(debug=False):
    nc = bass.Bass("TRN2", target_bir_lowering=False)
    I = {k: nc.dram_tensor(k, shp, F32, kind="ExternalInput").ap() for k, shp in INPUT_SHAPES.items()}

    def dint(name, shape, dt=F32):
        return nc.dram_tensor(name, shape, dt).ap()

    def ddbg(name, shape, dt=F32):
        if debug:
            return nc.dram_tensor(name, shape, dt, kind="ExternalOutput").ap()
        return dint(name, shape, dt)
    outT = nc.dram_tensor("outT", [2048, T], F32, kind="ExternalOutput").ap()
    P = Prog(nc)
    W = []
    for l in range(2):
        wgu_sh = dint("wgu_sh%d" % l, [8 * 128, 16384], BF16); wd_sh = dint("wd_sh%d" % l, [8 * 128, 8192], BF16)
        wguF = dint("wguF%d" % l, [64 * 128, 16384], BF16); wdF = dint("wdF%d" % l, [64 * 128, 8192], BF16)
        stage_moe_wprep(P, I["m%dwgus" % l], I["m%dwds" % l], wgu_sh, wd_sh, wguF, wdF, l)
        W.append((wguF, wdF))
    xbuf = dint("xbuf", [NBLK * 128, 2048], BF16); ybuf = dint("ybuf", [NBLK * 128, 2048])
    qT_d = dint("qT_d", [8, 128, TH], BF16); kT_d = dint("kT_d", [2, 128, TH], BF16); v_d = dint("v_d", [TH, 256], BF16)
    uT_d = dint("uT_d", [1024, TH], BF16); kmT_d = dint("kmT_d", [2, 128, 16], BF16); vm_d = dint("vm_d", [16, 256], BF16)
    aT_d = dint("aT_d", [8, 128, T], BF16)
    end_loc = dint("end_loc", [256, 64]); end_all = dint("end_all", [8 * 256, 64]); end_fin = dint("end_fin", [256, 64])
    yF_d = dint("yF_d", [1024, T]); yB_d = dint("yB_d", [1024, T]); sT_d = dint("sT_d", [1024, T], BF16)
    h1T = ddbg("h1T", [2048, T]); h2T = ddbg("h2T", [2048, T]); h3T = ddbg("h3T", [2048, T])
    stage_inproj_ab(P, I["hT0"], I["metaT"], I["w_in"], I["ropeC"], I["ropeS"], I["ropeCm"], I["ropeSm"], I["perm"],
                    qT_d, kT_d, v_d, uT_d, kmT_d, vm_d)
    e2 = lambda a: a.rearrange("(d p) f -> d p f", d=2)
    stage_s5_scan(P, uT_d, I["lamT"], I["lamR"], I["Bl"], I["Cl"], None, e2(end_loc), None, None, False)
    P.collective("AllGather", [end_loc], [end_all], reads=[], writes=["end_all"])
    P.barrier()
    ends4 = end_all.rearrange("(c d p) f -> c d p f", c=8, d=2)
    stage_s5_scan(P, uT_d, I["lamT"], I["lamR"], I["Bl"], I["Cl"], (ends4, I["fm"], I["bm"]), e2(end_fin), yF_d, yB_d, True)
    stage_s5_glu(P, uT_d, yF_d, yB_d, I["dsk"], I["wglu"], I["bglu"], sT_d)
    stage_attn(P, qT_d, kT_d, v_d, kmT_d, vm_d, I["kvalid"], I["maskL"], I["maskR"], I["sinks"], aT_d)
    parts = [aT_d[h] for h in range(8)] + [sT_d[j * 128:(j + 1) * 128, :] for j in range(8)]
    stage_outproj_ln(P, parts, I["w_out0"], I["ident"], I["m0g1"], I["m0b1"], I["hT0"][:, 128:128 + T], h1T)
    stage_moe_ln(P, h1T, I["m0wr"], I["m0br"], W[0][0], W[0][1], I["ident"], I["m0g2"], I["m0b2"], I["lstrict"], xbuf, ybuf, h2T, 0)
    q1T_d = dint("q1T_d", [2048, T], BF16); fT_d = dint("fT_d", [2, 2048, T]); gT_d = dint("gT_d", [2048, T], BF16); v1_d = dint("v1_d", [T, 2048], BF16)
    lbT_d = dint("lbT_d", [128, 16])
    S_loc = dint("S_loc", [256, 2048]); D_loc = dint("D_loc", [256, 16]); S_all = dint("S_all", [8 * 256, 2048]); D_all = dint("D_all", [8 * 256, 16])
    S_in = dint("S_in", [256, 2048]); S_fin = dint("S_fin", [256, 2048]); D_fin = dint("D_fin", [256, 16])
    oF_d = dint("oF_d", [2048, T]); oB_d = dint("oB_d", [2048, T]); oT_d = dint("oT_d", [2048, T], BF16)
    stage_lb(P, I["lgT"], lbT_d)
    stage_inproj_c(P, h2T, I["w_in_c"], q1T_d, fT_d, gT_d, v1_d)
    stage_hgrn_scan(P, q1T_d, fT_d, v1_d, lbT_d, I["ident"], I["maskR"], I["maskL"], None, e2(S_loc), e2(D_loc), None, None, False)
    P.collective("AllGather", [S_loc], [S_all], reads=[], writes=["S_all"])
    P.collective("AllGather", [D_loc], [D_all], reads=[], writes=["D_all"])
    P.barrier()
    stage_hgrn_carry(P, S_all.rearrange("(c d p) f -> c d p f", c=8, d=2), D_all.rearrange("(c d p) f -> c d p f", c=8, d=2),
                     I["fm"], I["bm"], e2(S_in))
    stage_hgrn_scan(P, q1T_d, fT_d, v1_d, lbT_d, I["ident"], I["maskR"], I["maskL"], e2(S_in), e2(S_fin), e2(D_fin), oF_d, oB_d, True)
    stage_hgrn_post(P, oF_d, oB_d, gT_d, I["ngT"], oT_d)
    parts1 = [oT_d[j * 128:(j + 1) * 128, :] for j in range(16)]
    stage_outproj_ln(P, parts1, I["w_out1"], I["ident"], I["m1g1"], I["m1b1"], h2T, h3T)
    stage_moe_ln(P, h3T, I["m1wr"], I["m1br"], W[1][0], W[1][1], I["ident"], I["m1g2"], I["m1b2"], I["lstrict"], xbuf, ybuf, outT, 1)
    return nc, P


def host_inputs(d):
    s5in = prep_s5_inputs(d)
    cc = prep_common_consts()
    common = {}
    common.update(s5in)
    common.update(cc)
    common["w_in"] = d["w_in_ab"][0]
    common["sinks"] = rep128(d["attn_sinks"][0])
    common["w_out0"] = d["w_out_ab"][0]
    common["w_out1"] = d["w_out_c"][0]
    common["w_in_c"] = d["w_in_c"][0]
    common["lgT"] = np.ascontiguousarray(d["hgrn_lb_logits"].reshape(2, 16, 128).transpose(2, 0, 1))
    common["ngT"] = np.ascontiguousarray(d["hgrn_norm_g"][0].reshape(16, 128).T)
    for l in range(2):
        m = prep_moe_inputs(d, l, "m%d" % l)
        m.pop("m%dwgu" % l)
        m.pop("m%dwd" % l)
        common.update(m)
    maps = []
    for c in range(NC):
        m = dict(common)
        m.update(prep_attn_inputs(d["x"], d["meta_tokens"], c))
        fm = np.zeros((128, 8), np.float32)
        bm = np.zeros((128, 8), np.float32)
        fm[:, :c] = 1.0
        bm[:, c + 1:] = 1.0
        m["fm"] = fm
        m["bm"] = bm
        for l in range(2):
            m["m%dwgus" % l] = d["moe_w_gate_up"][l, 8 * c:8 * c + 8].reshape(8 * 2048, 1024)
            m["m%dwds" % l] = d["moe_w_down"][l, 8 * c:8 * c + 8].reshape(8 * 512, 2048)
        maps.append({k: np.ascontiguousarray(v, dtype=np.float32) for k, v in m.items() if k in INPUT_SHAPES})
    return maps


def kernel(**inputs):
    d = {k: np.asarray(v) for k, v in inputs.items()}
    maps = host_inputs(d)
    nc, P = build_program(debug=False)
    res = run_bass_kernel_spmd(nc, maps, core_ids=list(range(NC)))
    full = np.concatenate([np.asarray(res.results[c]["outT"]).T for c in range(NC)], axis=0)
    return np.ascontiguousarray(full[16:][None].astype(np.float32))
```
